# Optimizing a Trainium2 kernel written in Bass

```python
import math
import jax, jax.numpy as jnp
from jax import lax
import numpy as np

D_MODEL = 1024
BATCH = 16
SEQ = 2048
DEPTH = 2

CHUNK = 64
Q_BLOCK = 128

MLA_HEADS = 8
MLA_NOPE = 64
MLA_ROPE = 32
MLA_V = 64
MLA_Q_RANK = 256
MLA_KV_RANK = 128
ROPE_THETA = 10000.0

CONV_GROUPS = 4
CONV_GROUP_WIDTH = 64
CONV_CH = CONV_GROUPS * CONV_GROUP_WIDTH
CONV_WIDTH = 3

SB_HEADS = 4
SB_HEAD_DIM = 64
SB_WIDTH = SB_HEADS * SB_HEAD_DIM

D_MIX = MLA_HEADS * MLA_V + CONV_CH + SB_WIDTH
IN_SIZES = (MLA_Q_RANK, MLA_KV_RANK, MLA_ROPE,
            CONV_CH, CONV_CH, CONV_CH,
            SB_WIDTH, SB_WIDTH, SB_WIDTH)
IN_COLS = sum(IN_SIZES)
IN_SPLITS = tuple(int(v) for v in np.cumsum(IN_SIZES)[:-1])

PEER_HEADS = 8
PEER_N_KEYS = 128
PEER_N_EXPERTS = PEER_N_KEYS * PEER_N_KEYS
PEER_D_KEY = 256
PEER_HALF = PEER_D_KEY // 2
PEER_TOPK = 16
PEER_TOKEN_BLOCK = 128

LN_EPS = 1e-5
RMS_EPS = 1e-6
DEEPNORM_ALPHA = (2.0 * DEPTH) ** 0.25
DEEPNORM_BETA = (8.0 * DEPTH) ** -0.25
NEG_INF = -1e30

kernel_name = "hybrid_mla_conv_stickbreak_peer_encoder"


def _layer_norm(x, gain, bias):
    xf = x.astype(jnp.float32)
    mu = jnp.mean(xf, axis=-1, keepdims=True)
    var = jnp.mean(jnp.square(xf - mu), axis=-1, keepdims=True)
    y = (xf - mu) * lax.rsqrt(var + LN_EPS) * gain.astype(jnp.float32) + bias.astype(jnp.float32)
    return y.astype(x.dtype)


def _modulate(x, shift, scale):
    xf = x.astype(jnp.float32)
    mu = jnp.mean(xf, axis=-1, keepdims=True)
    var = jnp.mean(jnp.square(xf - mu), axis=-1, keepdims=True)
    y = ((xf - mu) * lax.rsqrt(var + LN_EPS)).astype(x.dtype)
    return y * (1.0 + scale[:, None, :]) + shift[:, None, :]


def _rms_norm(x, gain):
    xf = x.astype(jnp.float32)
    y = xf * lax.rsqrt(jnp.mean(jnp.square(xf), axis=-1, keepdims=True) + RMS_EPS)
    return (y * gain.astype(jnp.float32)).astype(x.dtype)


def _rope(x, positions):
    half = x.shape[-1] // 2
    inv_freq = ROPE_THETA ** (-jnp.arange(half, dtype=jnp.float32) / half)
    ang = positions.astype(jnp.float32)[..., None] * inv_freq
    cos = jnp.cos(ang)[:, :, None, :]
    sin = jnp.sin(ang)[:, :, None, :]
    xf = x.astype(jnp.float32)
    x1, x2 = xf[..., :half], xf[..., half:]
    out = jnp.concatenate([x1 * cos - x2 * sin, x2 * cos + x1 * sin], axis=-1)
    return out.astype(x.dtype)


def _mla_attention(q_nope, q_rope, k_nope, k_rope, v):
    S = q_nope.shape[1]
    scale = (MLA_NOPE + MLA_ROPE) ** -0.5
    outs = []
    for i in range(S // Q_BLOCK):
        q0, q1 = i * Q_BLOCK, (i + 1) * Q_BLOCK
        kv_end = q1
        s = (jnp.einsum('bqhd,bkhd->bhqk', q_nope[:, q0:q1], k_nope[:, :kv_end])
             + jnp.einsum('bqhr,bkr->bhqk', q_rope[:, q0:q1], k_rope[:, :kv_end]))
        s = s.astype(jnp.float32) * scale
        q_chunk = (q0 + jnp.arange(Q_BLOCK)) // CHUNK
        k_chunk = jnp.arange(kv_end) // CHUNK
        mask = k_chunk[None, :] <= q_chunk[:, None]
        p = jax.nn.softmax(jnp.where(mask, s, NEG_INF), axis=-1)
        outs.append(jnp.einsum('bhqk,bkhd->bqhd', p.astype(v.dtype), v[:, :kv_end]))
    return jnp.concatenate(outs, axis=1)


def _stick_breaking(q, k, v):
    S = q.shape[1]
    scale = SB_HEAD_DIM ** -0.5
    outs = []
    for i in range(S // Q_BLOCK):
        q0, q1 = i * Q_BLOCK, (i + 1) * Q_BLOCK
        kv_end = q1
        z = jnp.einsum('bqhd,bkhd->bhqk', q[:, q0:q1], k[:, :kv_end]).astype(jnp.float32) * scale
        q_pos = q0 + jnp.arange(Q_BLOCK)
        k_pos = jnp.arange(kv_end)
        strict = k_pos[None, :] < q_pos[:, None]
        log_beta = jax.nn.log_sigmoid(z)
        log_one_minus = jnp.where(strict, jax.nn.log_sigmoid(-z), 0.0)
        after = lax.cumsum(log_one_minus, axis=3, reverse=True) - log_one_minus
        w = jnp.where(strict, jnp.exp(log_beta + after), 0.0)
        outs.append(jnp.einsum('bhqk,bkhd->bqhd', w.astype(v.dtype), v[:, :kv_end]))
    return jnp.concatenate(outs, axis=1)


def _short_conv(h, gate_b, gate_c, conv_w):
    u = gate_c * h
    S = u.shape[1]
    u_pad = jnp.pad(u, ((0, 0), (CONV_WIDTH - 1, 0), (0, 0)))
    y = conv_w[0] * u_pad[:, 0:S]
    for tap in range(1, CONV_WIDTH):
        y = y + conv_w[tap] * u_pad[:, tap:tap + S]
    return gate_b * y


def _hybrid_mixer(h, positions, w_in, q_norm_g, kv_norm_g, w_uq, w_ukv, conv_w, w_o):
    Bb, S, _ = h.shape
    proj = h @ w_in
    q_lat, kv_lat, k_rope, c_b, c_c, c_h, sb_q, sb_k, sb_v = jnp.split(proj, IN_SPLITS, axis=-1)
    q = (_rms_norm(q_lat, q_norm_g) @ w_uq).reshape(Bb, S, MLA_HEADS, MLA_NOPE + MLA_ROPE)
    q_nope = q[..., :MLA_NOPE]
    q_rope = _rope(q[..., MLA_NOPE:], positions)
    kv = (_rms_norm(kv_lat, kv_norm_g) @ w_ukv).reshape(Bb, S, MLA_HEADS, MLA_NOPE + MLA_V)
    k_nope, v = kv[..., :MLA_NOPE], kv[..., MLA_NOPE:]
    k_rope = _rope(k_rope[:, :, None, :], positions)[:, :, 0, :]
    mla_o = _mla_attention(q_nope, q_rope, k_nope, k_rope, v).reshape(Bb, S, MLA_HEADS * MLA_V)
    conv_o = _short_conv(c_h, c_b, c_c, conv_w)
    shp = (Bb, S, SB_HEADS, SB_HEAD_DIM)
    sb_o = _stick_breaking(sb_q.reshape(shp), sb_k.reshape(shp), sb_v.reshape(shp)).reshape(Bb, S, SB_WIDTH)
    return jnp.concatenate([mla_o, conv_o, sb_o], axis=-1) @ w_o


def _peer(h, wq, sub_k1, sub_k2, u_tab, v_tab):
    Bb, S, D = h.shape
    q = (h @ wq).reshape(Bb, S, PEER_HEADS, 2, PEER_HALF)
    s1 = jnp.einsum('bshd,hnd->bshn', q[..., 0, :], sub_k1).astype(jnp.float32)
    s2 = jnp.einsum('bshd,hnd->bshn', q[..., 1, :], sub_k2).astype(jnp.float32)
    v1, i1 = lax.top_k(s1, PEER_TOPK)
    v2, i2 = lax.top_k(s2, PEER_TOPK)
    cand_s = (v1[..., :, None] + v2[..., None, :]).reshape(Bb, S, PEER_HEADS, PEER_TOPK * PEER_TOPK)
    cand_e = (i1[..., :, None] * PEER_N_KEYS + i2[..., None, :]).reshape(Bb, S, PEER_HEADS, PEER_TOPK * PEER_TOPK)
    top_s, pos = lax.top_k(cand_s, PEER_TOPK)
    experts = jnp.take_along_axis(cand_e, pos, axis=-1)
    g = jax.nn.softmax(top_s, axis=-1)
    T = Bb * S
    nblk = T // PEER_TOKEN_BLOCK
    hf = h.reshape(nblk, PEER_TOKEN_BLOCK, D)
    ef = experts.reshape(nblk, PEER_TOKEN_BLOCK, PEER_HEADS * PEER_TOPK)
    gf = g.reshape(nblk, PEER_TOKEN_BLOCK, PEER_HEADS * PEER_TOPK).astype(h.dtype)

    def token_block(args):
        hb, eb, gb = args
        u = jnp.take(u_tab, eb, axis=0)
        act = jax.nn.gelu(jnp.einsum('tkd,td->tk', u, hb), approximate=False)
        vv = jnp.take(v_tab, eb, axis=0)
        return jnp.einsum('tk,tkd->td', gb * act, vv)

    out = lax.map(token_block, (hf, ef, gf))
    return out.reshape(Bb, S, D)


def setup_inputs(seed: int = 0) -> dict:
    key = jax.random.key(seed)
    ks = jax.random.split(key, 24)
    D, L = D_MODEL, DEPTH

    def nrm(k, shape, std):
        return jax.random.normal(k, shape, jnp.float32) * std

    x = nrm(ks[0], (BATCH, SEQ, D), 1.0)
    c = nrm(ks[1], (BATCH, D), 1.0)
    offset = jax.random.randint(ks[2], (BATCH, 1), 0, 4096, dtype=jnp.int32)
    positions = offset + jnp.arange(SEQ, dtype=jnp.int32)[None, :]
    ada_w = nrm(ks[3], (L, D, 6 * D), 0.5 * D ** -0.5)
    ada_b = nrm(ks[4], (L, 6 * D), 0.02)
    w_in = nrm(ks[5], (L, D, IN_COLS), D ** -0.5)
    q_norm_g = 1.0 + nrm(ks[6], (L, MLA_Q_RANK), 0.02)
    kv_norm_g = 1.0 + nrm(ks[7], (L, MLA_KV_RANK), 0.02)
    w_uq = nrm(ks[8], (L, MLA_Q_RANK, MLA_HEADS * (MLA_NOPE + MLA_ROPE)), MLA_Q_RANK ** -0.5)
    w_ukv = nrm(ks[9], (L, MLA_KV_RANK, MLA_HEADS * (MLA_NOPE + MLA_V)), MLA_KV_RANK ** -0.5)
    conv_w = nrm(ks[10], (L, CONV_WIDTH, CONV_CH), CONV_WIDTH ** -0.5)
    w_o = nrm(ks[11], (L, D_MIX, D), DEEPNORM_BETA * D_MIX ** -0.5)
    ln1_g = 1.0 + nrm(ks[12], (L, D), 0.02)
    ln1_b = nrm(ks[13], (L, D), 0.02)
    peer_wq = nrm(ks[14], (L, D, PEER_HEADS * PEER_D_KEY), D ** -0.5)
    peer_k1 = nrm(ks[15], (L, PEER_HEADS, PEER_N_KEYS, PEER_HALF), PEER_HALF ** -0.5)
    peer_k2 = nrm(ks[16], (L, PEER_HEADS, PEER_N_KEYS, PEER_HALF), PEER_HALF ** -0.5)
    peer_u = nrm(ks[17], (L, PEER_N_EXPERTS, D), D ** -0.5)
    peer_v = nrm(ks[18], (L, PEER_N_EXPERTS, D), DEEPNORM_BETA * PEER_HEADS ** -0.5)
    ln2_g = 1.0 + nrm(ks[19], (L, D), 0.02)
    ln2_b = nrm(ks[20], (L, D), 0.02)
    return {"x": x, "c": c, "positions": positions, "ada_w": ada_w, "ada_b": ada_b,
            "w_in": w_in, "q_norm_g": q_norm_g, "kv_norm_g": kv_norm_g, "w_uq": w_uq,
            "w_ukv": w_ukv, "conv_w": conv_w, "w_o": w_o, "ln1_g": ln1_g, "ln1_b": ln1_b,
            "peer_wq": peer_wq, "peer_k1": peer_k1, "peer_k2": peer_k2, "peer_u": peer_u,
            "peer_v": peer_v, "ln2_g": ln2_g, "ln2_b": ln2_b}


def reference(x, c, positions, ada_w, ada_b, w_in, q_norm_g, kv_norm_g, w_uq, w_ukv, conv_w,
              w_o, ln1_g, ln1_b, peer_wq, peer_k1, peer_k2, peer_u, peer_v, ln2_g, ln2_b):
    c_act = jax.nn.silu(c)
    for l in range(DEPTH):
        mod = c_act @ ada_w[l] + ada_b[l]
        sh1, sc1, g1, sh2, sc2, g2 = jnp.split(mod, 6, axis=-1)
        h = _modulate(x, sh1, sc1)
        mix = _hybrid_mixer(h, positions, w_in[l], q_norm_g[l], kv_norm_g[l], w_uq[l], w_ukv[l],
                            conv_w[l], w_o[l])
        x = _layer_norm(DEEPNORM_ALPHA * x + (1.0 + g1)[:, None, :] * mix, ln1_g[l], ln1_b[l])
        h = _modulate(x, sh2, sc2)
        ffn = _peer(h, peer_wq[l], peer_k1[l], peer_k2[l], peer_u[l], peer_v[l])
        x = _layer_norm(DEEPNORM_ALPHA * x + (1.0 + g2)[:, None, :] * ffn, ln2_g[l], ln2_b[l])
    return x
```

```python
from contextlib import ExitStack

import numpy as np
import concourse.bass as bass
import concourse.mybir as mybir
from concourse.bass_utils import run_bass_kernel_spmd

F32 = mybir.dt.float32
BF16 = mybir.dt.bfloat16
I32 = mybir.dt.int32
U32 = mybir.dt.uint32
ALU = mybir.AluOpType
AF = mybir.ActivationFunctionType
AX = mybir.AxisListType

L = 2
D = 1024
ALPHA = float((2.0 * L) ** 0.25)
LN_EPS = 1e-5
RMS_EPS = 1e-6
NE = 16384

ENGS = ("pe", "act", "dve", "pool", "sp")
EPOCH = 24000
NDS = 40
NDS_SP = 26


class Op:
    __slots__ = ("eng", "fn", "waits", "idx", "needs_inc", "semval", "epoch",
                 "is_dma", "slot", "target", "snap", "line")

    def __init__(self, eng, fn, is_dma=False):
        self.eng = eng
        self.fn = fn
        self.waits = []
        self.idx = 0
        self.needs_inc = False
        self.semval = 0
        self.epoch = 0
        self.is_dma = is_dma
        self.slot = -1
        self.target = 0
        self.snap = None


class _Res:
    __slots__ = ("w", "readers", "dreaders")

    def __init__(self):
        self.w = None
        self.readers = {}
        self.dreaders = []


class Prog:
    def __init__(self):
        self.ops = []
        self.count = {e: 0 for e in ENGS}
        self.res = {}
        self.known = {e: {} for e in ENGS}
        self.known_shared = {e: False for e in ENGS}
        self.slot_last = [None] * NDS
        self.slot_target = [0] * NDS
        self.next_slot = {}

    def _merge(self, eng, other, extra_stream, extra_val):
        k = self.known[eng]
        if self.known_shared[eng]:
            k = dict(k)
            self.known[eng] = k
            self.known_shared[eng] = False
        for s, v in other.items():
            if k.get(s, 0) < v:
                k[s] = v
        if k.get(extra_stream, 0) < extra_val:
            k[extra_stream] = extra_val

    def _add_wait(self, o, d):
        eng = o.eng
        k = self.known[eng]
        if d.is_dma:
            if k.get(("d", d.slot), 0) >= d.target:
                return
            self._merge(eng, d.snap, ("d", d.slot), d.target)
        else:
            if k.get(d.eng, 0) >= d.idx:
                return
            self._merge(eng, d.snap, d.eng, d.idx)
        d.needs_inc = True
        o.waits.append(d)

    def _rs(self, key):
        r = self.res.get(key)
        if r is None:
            r = _Res()
            self.res[key] = r
        return r

    def op(self, eng, fn, reads=(), writes=(), dma=False, extra_deps=()):
        o = Op(eng, fn, is_dma=dma)
        if getattr(self, "trace_lines", False):
            import sys as _s
            fr = _s._getframe(1)
            while fr.f_code.co_name in ("op", "mm", "tr", "act", "tt", "ts", "stt", "cp", "red", "memset", "dma", "proj", "ln_stats"):
                fr = fr.f_back
            o.line = fr.f_lineno
        self.count[eng] += 1
        o.idx = self.count[eng]
        raw = []
        other = []
        ex = [k for k in reads if isinstance(k, str) and (k[0] == "B" or k == "mp")]
        if ex:
            reads = [k for k in reads if k not in ex]
            writes = list(writes) + ex
        for key in reads:
            r = self._rs(key)
            if r.w is not None:
                raw.append(r.w)
        for key in writes:
            r = self._rs(key)
            if r.w is not None:
                other.append(r.w)
            other.extend(r.readers.values())
            other.extend(r.dreaders)
        raw.extend(extra_deps)
        if dma:
            lo, hi = (0, NDS_SP) if eng == "sp" else (NDS_SP, NDS)
            s = self.next_slot.get(eng, lo)
            self.next_slot[eng] = lo + (s + 1 - lo) % (hi - lo)
            o.slot = s
            if self.slot_last[s] is not None:
                raw.append(self.slot_last[s])
            self.slot_target[s] += 16
            o.target = self.slot_target[s]
            self.slot_last[s] = o
        seen = set()
        for d in raw:
            if id(d) in seen or d is o:
                continue
            seen.add(id(d))
            if (not d.is_dma) and d.eng == eng:
                if eng == "pe":
                    continue
                if self.known[eng].get(("self", eng), 0) >= d.idx:
                    continue
                d.needs_inc = True
                o.waits.append(d)
                self._merge(eng, {}, ("self", eng), d.idx)
            else:
                self._add_wait(o, d)
        for d in other:
            if id(d) in seen or d is o:
                continue
            seen.add(id(d))
            if (not d.is_dma) and d.eng == eng:
                continue
            self._add_wait(o, d)
        self.known_shared[eng] = True
        o.snap = self.known[eng]
        for key in reads:
            r = self._rs(key)
            if dma:
                r.dreaders.append(o)
            else:
                r.readers[eng] = o
        for key in writes:
            r = self._rs(key)
            r.w = o
            r.readers = {}
            r.dreaders = []
        self.ops.append(o)
        return o

    def barrier(self):
        last = {}
        for o in self.ops:
            if not o.is_dma and o.fn is not None:
                last[o.eng] = o
        deps = list(last.values()) + [d for d in self.slot_last if d is not None]
        for e in ENGS:
            self.op(e, None, extra_deps=deps)

    def emit(self, nc):
        n_epochs = {}
        per = {e: [] for e in ENGS}
        for o in self.ops:
            per[o.eng].append(o)
        for e in ENGS:
            cnt = 0
            for o in per[e]:
                if o.is_dma:
                    continue
                if o.needs_inc:
                    cnt += 1
                    o.epoch = (cnt - 1) // EPOCH
                    o.semval = (cnt - 1) % EPOCH + 1
            n_epochs[e] = (cnt + EPOCH - 1) // EPOCH
        with ExitStack() as es:
            sems = {}
            for e in ENGS:
                for ep in range(n_epochs[e]):
                    sems[(e, ep)] = es.enter_context(nc.semaphore(f"s_{e}_{ep}"))
            dsems = [es.enter_context(nc.semaphore(f"d_{i}")) for i in range(NDS)]
            block = es.enter_context(nc.Block())

            max_ops = getattr(self, "max_ops", None)
            if max_ops is not None:
                keep = set(id(o) for o in self.ops[:max_ops])
                per = {e: [o for o in per[e] if id(o) in keep] for e in ENGS}

            def run(engname):
                def body(e):
                    for o in per[engname]:
                        for d in o.waits:
                            if d.is_dma:
                                e.wait_ge(dsems[d.slot], d.target)
                            else:
                                e.wait_ge(sems[(d.eng, d.epoch)], d.semval)
                        if o.fn is None:
                            continue
                        ins = o.fn(e)
                        if o.is_dma:
                            ins.then_inc(dsems[o.slot], 16)
                        elif o.needs_inc:
                            ins.then_inc(sems[(engname, o.epoch)], 1)
                return body

            block.tensor(run("pe"))
            block.scalar(run("act"))
            block.vector(run("dve"))
            block.gpsimd(run("pool"))
            block.sync(run("sp"))


C_ID = 0
C_IOTA = 128
C_TRI = 256
C_MASK = 384
C_ONES = 512
C_IOTA16 = 640
C_INVF = 656
C_ONE1 = 657
NCONST = 658


def make_consts():
    c = np.zeros((128, NCONST), np.float32)
    c[:, C_ID:C_ID + 128] = np.eye(128)
    c[:, C_IOTA:C_IOTA + 128] = np.arange(128)[None, :]
    j = np.arange(128)[:, None]
    k = np.arange(128)[None, :]
    c[:, C_TRI:C_TRI + 128] = (j > k)
    c[:, C_MASK:C_MASK + 128] = (j < k)
    c[:, C_ONES:C_ONES + 128] = 1.0
    c[:, C_IOTA16:C_IOTA16 + 16] = np.arange(16)[None, :]
    inv = (10000.0 ** (-np.arange(16, dtype=np.float32) / 16.0)).astype(np.float32)
    for p in range(64, 96):
        c[p, C_INVF] = inv[(p - 64) % 16]
    c[:64, C_ONE1] = 1.0
    return c


class K:
    def __init__(self, nseq, S, layers=L, stop_after=None, dbg=False):
        self.nseq, self.S = nseq, S
        self.T = nseq * S
        self.NB = S // 128
        self.layers = layers
        self.stop_after = stop_after
        self.nc = bass.Bass("TRN2", target_bir_lowering=False)
        self.P = Prog()
        self.dbg = dbg
        self.dbg_out = {}

    def mm(self, out, lhsT, rhs, start, stop, R, W):
        return self.P.op("pe", lambda e: e.matmul(out, lhsT, rhs, start=start, stop=stop), R, W)

    def tr(self, out, in_, ident, R, W):
        return self.P.op("pe", lambda e: e.transpose(out=out, in_=in_, identity=ident), R, W)

    def act(self, out, in_, func, R, W, bias=None, scale=1.0):
        if bias is None:
            return self.P.op("act", lambda e: e.activation(out=out, in_=in_, func=func, scale=scale), R, W)
        return self.P.op("act", lambda e: e.activation(out=out, in_=in_, func=func, bias=bias, scale=scale), R, W)

    def tt(self, eng, out, in0, in1, op, R, W):
        return self.P.op(eng, lambda e: e.tensor_tensor(out=out, in0=in0, in1=in1, op=op), R, W)

    def ts(self, eng, out, in0, s1, s2, op0, op1, R, W):
        if s2 is None:
            return self.P.op(eng, lambda e: e.tensor_scalar(out=out, in0=in0, scalar1=s1, scalar2=None, op0=op0), R, W)
        return self.P.op(eng, lambda e: e.tensor_scalar(out=out, in0=in0, scalar1=s1, scalar2=s2, op0=op0, op1=op1), R, W)

    def stt(self, out, in0, scalar, in1, op0, op1, R, W):
        return self.P.op("dve", lambda e: e.scalar_tensor_tensor(out=out, in0=in0, scalar=scalar, in1=in1, op0=op0, op1=op1), R, W)

    def cp(self, eng, out, in_, R, W):
        if eng == "act":
            return self.P.op("act", lambda e: e.activation(out=out, in_=in_, func=AF.Copy), R, W)
        return self.P.op(eng, lambda e: e.tensor_copy(out=out, in_=in_), R, W)

    def red(self, out, in_, R, W, op=ALU.add):
        return self.P.op("dve", lambda e: e.tensor_reduce(out=out, in_=in_, axis=AX.X, op=op), R, W)

    def memset(self, eng, ap, val, W):
        return self.P.op(eng, lambda e: e.memset(ap, val), (), W)

    def dma(self, q, out, in_, R, W, slow=False):
        if slow:
            return self.P.op(q, lambda e: e.dma_start(out=out, in_=in_, allow_slow_non_contiguous=True), R, W, dma=True)
        return self.P.op(q, lambda e: e.dma_start(out=out, in_=in_), R, W, dma=True)

    def din(self, name, shape, dt=F32):
        return self.nc.dram_tensor(name, list(shape), dt, kind="ExternalInput").ap()

    def dscr(self, name, shape, dt):
        return self.nc.dram_tensor(name, list(shape), dt).ap()

    def declare(self):
        T, S, nseq = self.T, self.S, self.nseq
        self.x = self.din("x", [T, D])
        self.c = self.din("c", [nseq, D])
        self.pos = self.din("pos", [nseq, S], I32)
        self.consts = self.din("consts", [128, NCONST])
        self.ada_w = self.din("ada_w", [L, D, 6 * D])
        self.ada_b = self.din("ada_b", [L, 6 * D])
        self.w_in = self.din("w_in", [L, D, 1952])
        self.q_norm_g = self.din("q_norm_g", [L, 256])
        self.kv_norm_g = self.din("kv_norm_g", [L, 128])
        self.w_uq = self.din("w_uq", [L, 256, 768])
        self.w_ukv_k = self.din("w_ukv_k", [L, 128, 512])
        self.w_ukv_v = self.din("w_ukv_v", [L, 128, 512])
        self.conv_w = self.din("conv_w", [L, 3, 256])
        self.w_o = self.din("w_o", [L, D, D])
        self.ln1_g = self.din("ln1_g", [L, D])
        self.ln1_b = self.din("ln1_b", [L, D])
        self.peer_wq = self.din("peer_wq", [L, D, 2048])
        self.peer_kT = self.din("peer_kT", [L, 128, 16 * 128])
        self.peer_uT = self.din("peer_uT", [L, D, NE])
        self.peer_v = self.din("peer_v", [L, NE, D])
        self.ln2_g = self.din("ln2_g", [L, D])
        self.ln2_b = self.din("ln2_b", [L, D])
        self.out = self.nc.dram_tensor("out", [T, D], F32, kind="ExternalOutput").ap()
        self.adaw16 = self.dscr("adaw16", [L, D, 6 * D], BF16)
        self.win16 = self.dscr("win16", [L, D, 1952], BF16)
        self.wuq16 = self.dscr("wuq16", [L, 256, 768], BF16)
        self.wo16 = self.dscr("wo16", [L, D, D], BF16)
        self.wq16 = self.dscr("wq16", [L, D, 2048], BF16)
        self.kT16 = self.dscr("kT16", [L, 128, 2048], BF16)
        self.uT16 = self.dscr("uT16", [L, 128, 8, NE], BF16)
        self.v16 = self.dscr("v16", [L, NE, D], BF16)
        self.uG = self.dscr("uG", [L, 32, 128, 8, 512], BF16)
        self.vG = self.dscr("vG", [L, 64, 128, 2, D], BF16)
        self.modd = self.dscr("modd", [L, nseq, 6 * D], F32)
        self.x1 = self.dscr("x1", [T, D], F32)
        self.x2 = self.dscr("x2", [T, D], F32)

    def plan_casts(self):
        self.cast_keys = {}
        self.cast_q = {}

        def add(group, dst, src):
            keys = self.cast_keys.setdefault(group, [])
            key = (group, len(keys))
            keys.append(key)
            self.cast_q.setdefault(group, []).append((dst, src, key))

        for l in range(self.layers):
            def cast2d(group, dst, src, rows, cols):
                cstep = min(cols, 4096)
                for r0 in range(0, rows, 128):
                    for c0 in range(0, cols, cstep):
                        c1 = min(cols, c0 + cstep)
                        add(group, dst[r0:r0 + 128, c0:c1], src[r0:r0 + 128, c0:c1])
            cast2d(("adaw16", l), self.adaw16[l], self.ada_w[l], D, 6 * D)
            cast2d(("win16", l), self.win16[l], self.w_in[l], D, 1952)
            cast2d(("wuq16", l), self.wuq16[l], self.w_uq[l], 256, 768)
            cast2d(("wo16", l), self.wo16[l], self.w_o[l], D, D)
            cast2d(("wq16", l), self.wq16[l], self.peer_wq[l], D, 2048)
            cast2d(("kT16", l), self.kT16[l], self.peer_kT[l], 128, 2048)
            for c0 in range(0, NE, 4096):
                for dk in range(8):
                    add(("uT16", l), self.uT16[l, :, dk, c0:c0 + 4096],
                        self.peer_uT[l, dk * 128:(dk + 1) * 128, c0:c0 + 4096])
            vs = self.peer_v[l].rearrange("(a b) d -> a (b d)", b=4)
            vd = self.v16[l].rearrange("(a b) d -> a (b d)", b=4)
            for r0 in range(0, NE // 4, 128):
                add(("v16", l), vd[r0:r0 + 128, :], vs[r0:r0 + 128, :])

    def flush_casts(self, *groups):
        for g in groups:
            for dst, src, key in self.cast_q.pop(g, []):
                self.dma("pool", dst, src, [], [key])

    def bg_stream(self, l):
        for g in (("uT16", l), ("v16", l)):
            for dst, src, key in self.cast_q.pop(g, []):
                self.dma("pool", dst, src, [], [key])
                yield
        for g in range(32):
            self.dma("sp", self.uG[l, g], self.uT16[l, :, :, g * 512:(g + 1) * 512], self.ck(("uT16", l)), [("uG", l, g)])
            yield
        for gv in range(64):
            self.dma("sp", self.vG[l, gv], self.v16[l, gv * 256:(gv + 1) * 256, :].rearrange("(c p) d -> p c d", p=128),
                     self.ck(("v16", l)), [("vG", l, gv)])
            yield
        if l + 1 < self.layers:
            for g in (("adaw16", l + 1), ("win16", l + 1), ("wuq16", l + 1), ("wo16", l + 1), ("wq16", l + 1), ("kT16", l + 1)):
                for dst, src, key in self.cast_q.pop(g, []):
                    self.dma("pool", dst, src, [], [key])
                    yield

    def pump_bg(self, k):
        g = getattr(self, "bg", None)
        if g is None:
            return
        for _ in range(k):
            try:
                next(g)
            except StopIteration:
                self.bg = None
                return

    def ck(self, group):
        return list(self.cast_keys[group])

    def load_consts(self, es):
        nc = self.nc
        self.cst = es.enter_context(nc.sbuf_tensor("cst", [128, NCONST], F32))
        self.cb16 = es.enter_context(nc.sbuf_tensor("cb16", [128, 640], BF16))
        self.dma("sp", self.cst[:], self.consts, [], ["cst"])
        self.cp("dve", self.cb16[:], self.cst[:, 0:640], ["cst"], ["cb16"])
        self.ident16 = self.cb16[:, C_ID:C_ID + 128]
        self.tri16 = self.cb16[:, C_TRI:C_TRI + 128]
        self.mask16 = self.cb16[:, C_MASK:C_MASK + 128]
        self.ones16 = self.cb16[:, C_ONES:C_ONES + 128]
        self.identf = self.cst[:, C_ID:C_ID + 128]
        self.onesf = self.cst[:, C_ONES:C_ONES + 128]
        self.maskf = self.cst[:, C_MASK:C_MASK + 128]

    def ln_stats(self, src, st, sq, Rsrc, pfx):
        self.red(st[:, 0:1], src, Rsrc, [pfx + "st"])
        self.act(sq, src, AF.Square, Rsrc, [pfx + "sq"])
        self.red(st[:, 1:2], sq, [pfx + "sq"], [pfx + "st"])
        self.ts("dve", st[:, 2:3], st[:, 0:1], 1.0 / D, None, ALU.mult, None, [pfx + "st"], [pfx + "st"])
        self.tt("dve", st[:, 3:4], st[:, 2:3], st[:, 2:3], ALU.mult, [pfx + "st"], [pfx + "st"])
        self.stt(st[:, 4:5], st[:, 1:2], 1.0 / D, st[:, 3:4], ALU.mult, ALU.subtract, [pfx + "st"], [pfx + "st"])
        self.act(st[:, 5:6], st[:, 4:5], AF.Ln, [pfx + "st"], [pfx + "st"], bias=LN_EPS)
        self.act(st[:, 5:6], st[:, 5:6], AF.Exp, [pfx + "st"], [pfx + "st"], scale=-0.5)
        self.stt(st[:, 6:7], st[:, 2:3], -1.0, st[:, 5:6], ALU.mult, ALU.mult, [pfx + "st"], [pfx + "st"])

    def mod_phase(self, l):
        nc, nseq = self.nc, self.nseq
        with ExitStack() as es:
            cT = es.enter_context(nc.sbuf_tensor(f"cT{l}", [128, 8, nseq], F32))
            cTa = es.enter_context(nc.sbuf_tensor(f"cTa{l}", [128, 8, nseq], BF16))
            aw = es.enter_context(nc.sbuf_tensor(f"aw{l}", [128, 8, 6 * D], BF16))
            ab = es.enter_context(nc.sbuf_tensor(f"ab{l}", [nseq, 6 * D], F32))
            mo = es.enter_context(nc.sbuf_tensor(f"mo{l}", [nseq, 6 * D], F32))
            mp = es.enter_context(nc.psum_tensor(f"mp{l}", [nseq, 512], F32))
            for b in range(nseq):
                self.dma("sp", cT[:, :, b], self.c[b].rearrange("(k p) -> p k", p=128), [], ["cT"], slow=True)
                self.dma("sp", ab[b:b + 1, :], self.ada_b[l:l + 1, :], [], ["ab"])
            self.act(cTa[:], cT[:], AF.Silu, ["cT"], ["cTa"])
            for k in range(8):
                self.dma("sp", aw[:, k, :], self.adaw16[l, k * 128:(k + 1) * 128, :], self.ck(("adaw16", l)), ["aw"])
            for g in range(12):
                for k in range(8):
                    self.mm(mp[:], cTa[:, k, :], aw[:, k, g * 512:(g + 1) * 512], k == 0, k == 7,
                            ["cTa", "aw"], ["mp"])
                self.tt("dve", mo[:, g * 512:(g + 1) * 512], mp[:], ab[:, g * 512:(g + 1) * 512], ALU.add,
                        ["mp", "ab"], ["mo"])
            for slot in (1, 2, 4, 5):
                self.ts("dve", mo[:, slot * D:(slot + 1) * D], mo[:, slot * D:(slot + 1) * D], 1.0, None, ALU.add, None,
                        ["mo"], ["mo"])
            self.dma("sp", self.modd[l], mo[:], ["mo"], [("modd", l)])

    def phase_a(self, l, xsrc, xsrc_key, xdst, xdst_key):
        nc, S, NB = self.nc, self.S, self.NB
        with ExitStack() as es:
            sb = lambda n, s, d: es.enter_context(nc.sbuf_tensor(f"{n}_a{l}", s, d))
            pst = lambda n, s, d=F32: es.enter_context(nc.psum_tensor(f"{n}_a{l}", s, d))
            win = sb("win", [128, 8, 1952], BF16)
            wkr = sb("wkr", [128, 8, 128], BF16)
            wkrr = sb("wkrr", [128, 8, 128], BF16)
            wuq = sb("wuq", [128, 2, 800], BF16)
            wuqr = sb("wuqr", [128, 2, 800], BF16)
            wk = sb("wk", [128, 512], BF16)
            wv = sb("wv", [128, 512], BF16)
            wtmp = sb("wtmp", [128, 1024], F32)
            wo = sb("wo", [128, 8, 1024], BF16)
            cw = sb("cw", [128, 2, 3], F32)
            gq = sb("gq", [128, 2], F32)
            gkv = sb("gkv", [128, 1], F32)
            lng = sb("lng", [128, D], F32)
            lnb = sb("lnb", [128, D], F32)
            scp = sb("scp", [128, D], F32)
            shp = sb("shp", [128, D], F32)
            gp = sb("gp", [128, D], F32)
            KT = sb("KT", [128, 8, S], BF16)
            Vaug = sb("Vaug", [128, NB, 8, 65], BF16)
            SKT = sb("SKT", [128, 2, S], BF16)
            SV = sb("SV", [128, NB, 256], BF16)
            uT = sb("uT", [128, 2, 130], F32)
            xt = sb("xt", [128, D], F32)
            tA = sb("tA", [128, D], F32)
            tB = sb("tB", [128, D], F32)
            st = sb("st", [128, 8], F32)
            hb = sb("hb", [128, D], BF16)
            hT = sb("hT", [128, 8, 128], BF16)
            qlat = sb("qlat", [128, 2, 128], BF16)
            sqq = sb("sqq", [128, 2, 128], F32)
            kvlat = sb("kvlat", [128, 128], BF16)
            sqkv = sb("sqkv", [128, 128], F32)
            rqb = sb("rqb", [128, 128], F32)
            rkvb = sb("rkvb", [128, 128], F32)
            rkvt = sb("rkvt", [128, 1], F32)
            posi = sb("posi", [128, 128], I32)
            ang = sb("ang", [128, 128], F32)
            angi = sb("angi", [128, 128], I32)
            angf = sb("angf", [128, 128], F32)
            CS1 = sb("CS1", [128, 128], F32)
            CS2 = sb("CS2", [128, 128], F32)
            q1 = sb("q1", [128, 4, 128], F32)
            q2 = sb("q2", [128, 4, 128], F32)
            QT = sb("QT", [128, 8, 128], BF16)
            krf = sb("krf", [128, 128], F32)
            krf2 = sb("krf2", [128, 128], F32)
            SQT = sb("SQT", [128, 2, 128], BF16)
            ccs = sb("ccs", [128, 2, 128], F32)
            yt = sb("yt", [128, 128], F32)
            PT = [sb(f"PT{j}", [128, 4, 128], BF16) for j in range(2)]
            rec = sb("rec", [128, 4, 1], F32)
            mix = sb("mix", [128, D], BF16)
            mixT = sb("mixT", [128, 8, 128], BF16)
            esb = sb("esb", [128, 4, 128], F32)
            lp = sb("lp", [128, 4, 128], F32)
            L1 = sb("L1", [128, 4, 128], BF16)
            Ls = [sb(f"Ls{j}", [128, 128], BF16) for j in range(2)]
            lg = sb("lg", [128, 4, 128], F32)
            WT = [sb(f"WT{j}", [128, 4, 128], BF16) for j in range(2)]
            B = [pst(f"B{j}", [128, 512]) for j in range(7)]
            BT = pst("BT", [128, 8, 128], BF16)
            Bk = [f"B{j}" for j in range(7)]

            for k in range(8):
                self.dma("sp", win[:, k, :], self.win16[l, k * 128:(k + 1) * 128, :], self.ck(("win16", l)), ["win"])
                self.dma("sp", wo[:, k, :], self.wo16[l, k * 128:(k + 1) * 128, :], self.ck(("wo16", l)), ["wo"])
            self.memset("pool", wuq[:], 0.0, ["wuq"])
            for k in range(2):
                self.dma("sp", wuq[:, k, 0:768], self.wuq16[l, k * 128:(k + 1) * 128, :], self.ck(("wuq16", l)), ["wuq"])
            self.dma("sp", gq[:], self.q_norm_g[l].rearrange("(k p) -> p k", p=128), [], ["gq"], slow=True)
            self.dma("sp", gkv[:], self.kv_norm_g[l].rearrange("(p o) -> p o", o=1), [], ["gkv"], slow=True)
            for tap in range(3):
                self.dma("sp", cw[:, :, tap], self.conv_w[l, tap].rearrange("(k p) -> p k", p=128), [], ["cw"], slow=True)
            self.dma("sp", lng[:], self.ln1_g[l].partition_broadcast(128), [], ["lng"])
            self.dma("sp", lnb[:], self.ln1_b[l].partition_broadcast(128), [], ["lnb"])
            self.memset("pool", wkr[:], 0.0, ["wkr"])
            self.memset("pool", wkrr[:], 0.0, ["wkrr"])
            self.cp("pool", wkr[:, :, 64:96], win[:, :, 384:416], ["win"], ["wkr"])
            self.ts("pool", wkrr[:, :, 64:80], win[:, :, 400:416], -1.0, None, ALU.mult, None, ["win"], ["wkrr"])
            self.cp("pool", wkrr[:, :, 80:96], win[:, :, 384:400], ["win"], ["wkrr"])
            for k in range(2):
                self.ts("dve", wuq[:, k, :], wuq[:, k, :], gq[:, k:k + 1], None, ALU.mult, None, ["wuq", "gq"], ["wuq"])
            self.memset("pool", wuqr[:], 0.0, ["wuqr"])
            wv4 = wuq[:, :, 0:768].rearrange("p k (h c) -> p k h c", c=96)
            wr4 = wuqr[:, :, 0:768].rearrange("p k (h c) -> p k h c", c=96)
            for k in range(2):
                self.ts("pool", wr4[:, k, :, 64:80], wv4[:, k, :, 80:96], -1.0, None, ALU.mult, None, ["wuq"], ["wuqr"])
                self.cp("pool", wr4[:, k, :, 80:96], wv4[:, k, :, 64:80], ["wuq"], ["wuqr"])
            self.dma("sp", wtmp[:, 0:512], self.w_ukv_k[l], [], ["wtmp"])
            self.dma("sp", wtmp[:, 512:1024], self.w_ukv_v[l], [], ["wtmp"])
            self.ts("dve", wk[:], wtmp[:, 0:512], gkv[:, 0:1], None, ALU.mult, None, ["wtmp", "gkv"], ["wk"])
            self.ts("dve", wv[:], wtmp[:, 512:1024], gkv[:, 0:1], None, ALU.mult, None, ["wtmp", "gkv"], ["wv"])

            inv_sqrt96 = float(96.0 ** -0.5)
            self.memset("pool", CS1[0:64, :], 1.0, ["CS1"])
            self.memset("pool", CS1[64:128, :], 0.0, ["CS1"])
            self.memset("pool", CS2[:, :], 0.0, ["CS2"])
            self.memset("pool", KT[64:128, :, :], 0.0, ["KT"])
            for b in range(self.nseq):
                self.dma("sp", shp[:], self.modd[l, b, 0:D].partition_broadcast(128), [("modd", l)], ["shp"])
                self.dma("sp", scp[:], self.modd[l, b, D:2 * D].partition_broadcast(128), [("modd", l)], ["scp"])
                self.dma("sp", gp[:], self.modd[l, b, 2 * D:3 * D].partition_broadcast(128), [("modd", l)], ["gp"])
                self.memset("pool", uT[:], 0.0, ["uT"])
                self.memset("pool", Vaug[:], 1.0, ["Vaug"])
                for i in range(NB):
                    t0 = b * S + i * 128
                    s0 = i * 128
                    sl = slice(s0, s0 + 128)
                    self.dma("sp", xt[:], xsrc[t0:t0 + 128, :], [xsrc_key], ["xt"])
                    self.ln_stats(xt[:], st, tA[:], ["xt"], "a")
                    self.act(tB[:], xt[:], AF.Identity, ["xt", "ast"], ["tB"], bias=st[:, 6:7], scale=st[:, 5:6])
                    self.tt("dve", tB[:], tB[:], scp[:], ALU.mult, ["tB", "scp"], ["tB"])
                    self.tt("dve", hb[:], tB[:], shp[:], ALU.add, ["tB", "shp"], ["hb"])
                    for k in range(8):
                        self.tr(BT[:, k, :], hb[:, k * 128:(k + 1) * 128], self.ident16, ["hb", "cb16"], ["BT"])
                    self.cp("act", hT[:], BT[:], ["BT"], ["hT"])
                    self.dma("sp", posi[64:96, :], self.pos[b, sl].partition_broadcast(32), [], ["posi"])
                    self.cp("dve", ang[64:96, :], posi[64:96, :], ["posi"], ["ang"])
                    self.ts("dve", ang[64:96, :], ang[64:96, :], self.cst[64:96, C_INVF:C_INVF + 1],
                            float(1.0 / (2 * np.pi)), ALU.mult, ALU.mult, ["ang", "cst"], ["ang"])
                    self.cp("dve", angi[64:96, :], ang[64:96, :], ["ang"], ["angi"])
                    self.cp("dve", angf[64:96, :], angi[64:96, :], ["angi"], ["angf"])
                    self.tt("dve", angf[64:96, :], ang[64:96, :], angf[64:96, :], ALU.subtract, ["ang", "angf"], ["angf"])
                    self.act(CS2[64:96, :], angf[64:96, :], AF.Sin, ["angf"], ["CS2"], scale=float(2 * np.pi))
                    self.ts("dve", ang[64:96, :], ang[64:96, :], 0.25, None, ALU.add, None, ["ang"], ["ang"])
                    self.cp("dve", angi[64:96, :], ang[64:96, :], ["ang"], ["angi"])
                    self.cp("dve", angf[64:96, :], angi[64:96, :], ["angi"], ["angf"])
                    self.tt("dve", angf[64:96, :], ang[64:96, :], angf[64:96, :], ALU.subtract, ["ang", "angf"], ["angf"])
                    self.act(CS1[64:96, :], angf[64:96, :], AF.Sin, ["angf"], ["CS1"], scale=float(2 * np.pi))
                    if i == 0 and b == 0:
                        pass

                    def proj(bank, slot, lhs_of_k, M):
                        for k in range(8):
                            self.mm(B[bank][0:M, slot * 128:(slot + 1) * 128], lhs_of_k(k), hT[:, k, :], k == 0, k == 7,
                                    ["hT", "win", "wkr", "wkrr"], [Bk[bank]])
                    wcol = lambda c0, w=128: (lambda k: win[:, k, c0:c0 + w])
                    proj(0, 0, wcol(0), 128)
                    proj(0, 1, wcol(128), 128)
                    proj(0, 2, wcol(256), 128)
                    proj(1, 0, lambda k: wkr[:, k, :], 128)
                    proj(1, 1, lambda k: wkrr[:, k, :], 128)
                    proj(1, 2, wcol(1184), 128)
                    proj(1, 3, wcol(1312), 128)
                    proj(2, 0, wcol(1440), 128)
                    proj(2, 1, wcol(1568), 128)
                    proj(2, 2, wcol(672), 128)
                    proj(2, 3, wcol(800), 128)
                    proj(3, 0, wcol(928), 128)
                    proj(3, 1, wcol(1056), 128)
                    proj(3, 2, wcol(416), 128)
                    proj(3, 3, wcol(544), 128)
                    for k in range(8):
                        self.mm(B[4][:, 0:256], hT[:, k, :], win[:, k, 1696:1952], k == 0, k == 7, ["hT", "win"], [Bk[4]])
                    b0v = B[0][:, 0:256].rearrange("p (c t) -> p c t", t=128)
                    self.cp("act", qlat[:], b0v, [Bk[0]], ["qlat"])
                    self.act(sqq[:], b0v, AF.Square, [Bk[0]], ["sqq"])
                    self.cp("act", kvlat[:], B[0][:, 256:384], [Bk[0]], ["kvlat"])
                    self.act(sqkv[:], B[0][:, 256:384], AF.Square, [Bk[0]], ["sqkv"])
                    self.cp("act", SQT[:], B[1][:, 256:512].rearrange("p (c t) -> p c t", t=128), [Bk[1]], ["SQT"])
                    self.cp("act", SKT[:, :, sl], B[2][:, 0:256].rearrange("p (c t) -> p c t", t=128), [Bk[2]], ["SKT"])
                    self.cp("act", SV[:, i, :], B[4][:, 0:256], [Bk[4]], ["SV"])
                    self.cp("act", ccs[:], B[2][:, 256:512].rearrange("p (c t) -> p c t", t=128), [Bk[2]], ["ccs"])
                    self.tt("dve", krf[:, :], B[1][:, 0:128], CS1[:, :], ALU.mult, [Bk[1], "CS1"], ["krf"])
                    self.tt("dve", krf2[:, :], B[1][:, 128:256], CS2[:, :], ALU.mult, [Bk[1], "CS2"], ["krf2"])
                    self.tt("pool", krf[64:96, :], krf[64:96, :], krf2[64:96, :], ALU.add, ["krf", "krf2"], ["krf"])
                    self.cp("pool", KT[64:96, :, sl], krf[64:96, :].unsqueeze(1).to_broadcast([32, 8, 128]), ["krf"], ["KT"])
                    self.tt("dve", uT[:, :, 2:130], ccs[:], B[3][:, 0:256].rearrange("p (c t) -> p c t", t=128), ALU.mult,
                            ["ccs", Bk[3]], ["uT"])
                    for c in range(2):
                        self.ts("dve", yt[:], uT[:, c, 2:130], cw[:, c, 2:3], None, ALU.mult, None, ["uT", "cw"], ["yt"])
                        self.stt(yt[:], uT[:, c, 1:129], cw[:, c, 1:2], yt[:], ALU.mult, ALU.add, ["uT", "cw", "yt"], ["yt"])
                        self.stt(yt[:], uT[:, c, 0:128], cw[:, c, 0:1], yt[:], ALU.mult, ALU.add, ["uT", "cw", "yt"], ["yt"])
                        self.tt("dve", mixT[:, 4 + c, :], yt[:], B[3][:, 256 + c * 128:384 + c * 128], ALU.mult,
                                ["yt", Bk[3]], ["mixT"])
                    self.cp("pool", uT[:, :, 0:2], uT[:, :, 128:130], ["uT"], ["uT"])
                    for c in range(2):
                        self.mm(B[5][:, 0:128], self.onesf, sqq[:, c, :], c == 0, c == 1, ["cst", "sqq"], [Bk[5]])
                    self.mm(B[5][:, 128:256], self.onesf, sqkv[:], True, True, ["cst", "sqkv"], [Bk[5]])
                    self.mm(B[5][:, 256:257], sqkv[:], self.onesf[:, 0:1], True, True, ["cst", "sqkv"], [Bk[5]])
                    self.act(rqb[:], B[5][:, 0:128], AF.Ln, [Bk[5]], ["rqb"], bias=RMS_EPS, scale=1.0 / 256)
                    self.act(rqb[:], rqb[:], AF.Exp, ["rqb"], ["rqb"], scale=-0.5)
                    self.act(rkvb[:], B[5][:, 128:256], AF.Ln, [Bk[5]], ["rkvb"], bias=RMS_EPS, scale=1.0 / 128)
                    self.act(rkvb[:], rkvb[:], AF.Exp, ["rkvb"], ["rkvb"], scale=-0.5)
                    self.act(rkvt[:], B[5][:, 256:257], AF.Ln, [Bk[5]], ["rkvt"], bias=RMS_EPS, scale=1.0 / 128)
                    self.act(rkvt[:], rkvt[:], AF.Exp, ["rkvt"], ["rkvt"], scale=-0.5)
                    for hg in range(2):
                        for hh in range(4):
                            h = hg * 4 + hh
                            for k in range(2):
                                self.mm(B[0][:, hh * 128:(hh + 1) * 128], wuq[:, k, h * 96:h * 96 + 128], qlat[:, k, :],
                                        k == 0, k == 1, ["wuq", "qlat"], [Bk[0]])
                            for k in range(2):
                                self.mm(B[1][:, hh * 128:(hh + 1) * 128], wuqr[:, k, h * 96:h * 96 + 128], qlat[:, k, :],
                                        k == 0, k == 1, ["wuqr", "qlat"], [Bk[1]])
                        v0 = B[0][:, :].rearrange("p (h t) -> p h t", t=128)
                        v1 = B[1][:, :].rearrange("p (h t) -> p h t", t=128)
                        bc = lambda a: a.unsqueeze(1).to_broadcast([128, 4, 128])
                        self.tt("dve", q1[:], v0, bc(CS1[:, :]), ALU.mult, [Bk[0], "CS1"], ["q1"])
                        self.tt("dve", q2[:], v1, bc(CS2[:, :]), ALU.mult, [Bk[1], "CS2"], ["q2"])
                        self.tt("pool", q1[:], q1[:], q2[:], ALU.add, ["q1", "q2"], ["q1"])
                        self.tt("pool", QT[:, hg * 4:(hg + 1) * 4, :], q1[:], bc(rqb[:, :]), ALU.mult,
                                ["q1", "rqb"], ["QT"])
                    for hg in range(2):
                        for hh in range(4):
                            h = hg * 4 + hh
                            self.mm(B[2][0:64, hh * 128:(hh + 1) * 128], wk[:, h * 64:(h + 1) * 64], kvlat[:], True, True,
                                    ["wk", "kvlat"], [Bk[2]])
                        self.tt("dve", KT[0:64, hg * 4:(hg + 1) * 4, sl], B[2][0:64, :].rearrange("p (h t) -> p h t", t=128),
                                rkvb[0:64, :].unsqueeze(1).to_broadcast([64, 4, 128]), ALU.mult, [Bk[2], "rkvb"], ["KT"])
                    self.mm(B[3][:, :], kvlat[:], wv[:], True, True, ["kvlat", "wv"], [Bk[3]])
                    self.act(Vaug[:, i, :, 0:64], B[3][:, :].rearrange("p (h c) -> p h c", c=64), AF.Identity, [Bk[3], "rkvt"],
                             ["Vaug"], scale=rkvt[:, 0:1])

                    nk = i + 1

                    def mla_gen():
                        gm = 0
                        for h in range(8):
                            ob = 5 + (h // 4)
                            hh = h % 4
                            ov = B[ob][:, 0:260].rearrange("p (h c) -> p h c", c=65)
                            for g0 in range(0, nk, 4):
                                kbs = list(range(g0, min(g0 + 4, nk)))
                                sbk = 3 + (gm % 2)
                                pt = PT[gm % 2]
                                ptk = f"PT{gm % 2}"
                                gm += 1
                                for j, kb in enumerate(kbs):
                                    self.mm(B[sbk][:, j * 128:(j + 1) * 128], KT[:, h, kb * 128:(kb + 1) * 128], QT[:, h, :],
                                            True, True, ["KT", "QT"], [Bk[sbk]])
                                n = len(kbs)
                                self.act(pt[:, 0:n, :], B[sbk][:, 0:n * 128].rearrange("p (j t) -> p j t", t=128), AF.Exp,
                                         [Bk[sbk]], [ptk], scale=inv_sqrt96)
                                if kbs[-1] == i:
                                    self.memset("pool", pt[64:128, n - 1, 0:64], 0.0, [ptk])
                                yield
                                for j, kb in enumerate(kbs):
                                    self.mm(ov[:, hh, :], pt[:, j, :], Vaug[:, kb, h, :], kb == 0, kb == i,
                                            [ptk, "Vaug"], [Bk[ob]])
                                yield
                            if hh == 3:
                                self.P.op("dve", (lambda ov=ov: lambda e: e.reciprocal(out=rec[:], in_=ov[:, :, 64:65]))(),
                                          [Bk[ob]], ["rec"])
                                self.tt("dve", mix[:, (h // 4) * 256:(h // 4 + 1) * 256].rearrange("p (h c) -> p h c", c=64),
                                        ov[:, :, 0:64], rec[:].to_broadcast([128, 4, 64]), ALU.mult, [Bk[ob], "rec"], ["mix"])
                                yield

                    def sb_gen():
                        lsi = 0
                        gs = 0
                        zb, ab_ = 0, 1
                        for h in range(4):
                            c = h // 2
                            po = (h % 2) * 64
                            first = True
                            groups = [list(range(g0, min(g0 + 4, nk))) for g0 in range(0, nk, 4)]
                            for kbs in reversed(groups):
                                n = len(kbs)
                                wt = WT[gs % 2]
                                wtk = f"WT{gs % 2}"
                                gs += 1
                                for j, kb in enumerate(kbs):
                                    self.mm(B[zb][:, j * 128:(j + 1) * 128], SKT[po:po + 64, c, kb * 128:(kb + 1) * 128],
                                            SQT[po:po + 64, c, :], True, True, ["SKT", "SQT"], [Bk[zb]])
                                yield
                                zv = B[zb][:, 0:n * 128].rearrange("p (j t) -> p j t", t=128)
                                self.act(esb[:, 0:n, :], zv, AF.Exp, [Bk[zb]], ["esb"], scale=-0.125)
                                self.act(lp[:, 0:n, :], esb[:, 0:n, :], AF.Ln, ["esb"], ["lp"], bias=1.0)
                                self.stt(L1[:, 0:n, :], zv, -0.125, lp[:, 0:n, :], ALU.mult, ALU.subtract, [Bk[zb], "lp"], ["L1"])
                                if kbs[-1] == i:
                                    self.tt("pool", L1[:, n - 1, :], L1[:, n - 1, :], self.mask16, ALU.mult, ["L1", "cb16"], ["L1"])
                                yield
                                for j in reversed(range(n)):
                                    kb = kbs[j]
                                    self.mm(B[ab_][:, j * 128:(j + 1) * 128], self.tri16, L1[:, j, :], True, kb == i,
                                            ["cb16", "L1"], [Bk[ab_]])
                                    if kb < i:
                                        self.mm(B[ab_][:, j * 128:(j + 1) * 128], self.ones16, Ls[lsi % 2][:], False, True,
                                                ["cb16", f"Ls{lsi % 2}"], [Bk[ab_]])
                                        self.tt("pool", Ls[(lsi + 1) % 2][:], Ls[lsi % 2][:], L1[:, j, :], ALU.add,
                                                [f"Ls{lsi % 2}", "L1"], [f"Ls{(lsi + 1) % 2}"])
                                    else:
                                        self.cp("pool", Ls[(lsi + 1) % 2][:], L1[:, j, :], ["L1"], [f"Ls{(lsi + 1) % 2}"])
                                    lsi += 1
                                yield
                                av = B[ab_][:, 0:n * 128].rearrange("p (j t) -> p j t", t=128)
                                self.tt("dve", lg[:, 0:n, :], av, lp[:, 0:n, :], ALU.subtract, [Bk[ab_], "lp"], ["lg"])
                                self.act(wt[:, 0:n, :], lg[:, 0:n, :], AF.Exp, ["lg"], [wtk])
                                if kbs[-1] == i:
                                    self.tt("pool", wt[:, n - 1, :], wt[:, n - 1, :], self.mask16, ALU.mult, [wtk, "cb16"], [wtk])
                                yield
                                for j in reversed(range(n)):
                                    kb = kbs[j]
                                    self.mm(B[2][:, h * 64:(h + 1) * 64], wt[:, j, :], SV[:, kb, h * 64:(h + 1) * 64],
                                            first, kb == 0, [wtk, "SV"], [Bk[2]])
                                    first = False
                                yield
                        self.cp("act", mix[:, 768:1024], B[2][:, 0:256], [Bk[2]], ["mix"])

                    gens = [mla_gen(), sb_gen()]
                    alive = [True, True]
                    while alive[0] or alive[1]:
                        for gi_, cnt in ((0, 1), (1, 1)):
                            for _ in range(cnt):
                                if alive[gi_]:
                                    try:
                                        next(gens[gi_])
                                    except StopIteration:
                                        alive[gi_] = False

                    for cch in (0, 1, 2, 3, 6, 7):
                        self.tr(BT[:, cch, :], mix[:, cch * 128:(cch + 1) * 128], self.ident16, ["mix", "cb16"], ["BT"])
                    self.cp("act", mixT[:, 0:4, :], BT[:, 0:4, :], ["BT"], ["mixT"])
                    self.cp("act", mixT[:, 6:8, :], BT[:, 6:8, :], ["BT"], ["mixT"])
                    for hf in range(2):
                        for k in range(8):
                            self.mm(B[hf][:, :], mixT[:, k, :], wo[:, k, hf * 512:(hf + 1) * 512], k == 0, k == 7,
                                    ["mixT", "wo"], [Bk[hf]])
                    for hf in range(2):
                        cs = slice(hf * 512, (hf + 1) * 512)
                        self.tt("dve", tA[:, cs], B[hf][:, :], gp[:, cs], ALU.mult, [Bk[hf], "gp"], ["tA"])
                    self.stt(tA[:], xt[:], ALPHA, tA[:], ALU.mult, ALU.add, ["xt", "tA"], ["tA"])
                    self.ln_stats(tA[:], st, tB[:], ["tA"], "a")
                    self.act(tB[:], tA[:], AF.Identity, ["tA", "ast"], ["tB"], bias=st[:, 6:7], scale=st[:, 5:6])
                    self.tt("dve", tB[:], tB[:], lng[:], ALU.mult, ["tB", "lng"], ["tB"])
                    self.tt("pool", tB[:], tB[:], lnb[:], ALU.add, ["tB", "lnb"], ["tB"])
                    self.dma("sp", xdst[t0:t0 + 128, :], tB[:], ["tB"], [xdst_key])
                    self.pump_bg(4)

    def phase_b(self, l, xsrc, xsrc_key, xdst, xdst_key):
        nc, S = self.nc, self.S
        TT = 256
        NT = S // TT
        G = 4
        NQ = 8
        with ExitStack() as es:
            sb = lambda n, s, d: es.enter_context(nc.sbuf_tensor(f"{n}_b{l}", s, d))
            pst = lambda n, s, d=F32: es.enter_context(nc.psum_tensor(f"{n}_b{l}", s, d))
            wqs = [sb(f"wqs{j}", [128, 8, 256], BF16) for j in range(2)]
            kT = sb("kT", [128, 16, 128], BF16)
            lng = sb("lng", [128, D], F32)
            lnb = sb("lnb", [128, D], F32)
            scp = sb("scp", [128, D], F32)
            shp = sb("shp", [128, D], F32)
            gp = sb("gp", [128, D], F32)
            xt = sb("xt", [128, D], F32)
            tB = sb("tB", [128, D], F32)
            st = sb("st", [128, 8], F32)
            hb = sb("hb", [128, D], BF16)
            hT = [sb(f"hT{j}", [128, 8, TT], BF16) for j in range(2)]
            qT = sb("qT", [128, 16, 128], BF16)
            s12 = sb("s12", [128, 16, 128], F32)
            cand = s12[:].rearrange("p (h x) n -> p h (x n)", x=2)
            mr = sb("mr", [128, 8, 128], F32)
            mrc = sb("mrc", [128, 2, 256], F32)
            v12 = sb("v12", [128, 16, 16], F32)
            i12 = sb("i12", [128, 16, 16], U32)
            i12f = sb("i12f", [128, 16, 16], F32)
            ts_ = sb("ts", [128, 8, 16], F32)
            pos = sb("pos", [128, 8, 16], U32)
            pa = sb("pa", [128, 8, 16], U32)
            pb = sb("pb", [128, 8, 16], U32)
            paf = sb("paf", [128, 128], F32)
            pbf = sb("pbf", [128, 128], F32)
            oh = sb("oh", [128, 32, 16], F32)
            sel = sb("sel", [128, 3, 128], F32)
            ssum = sb("ssum", [128, 8], F32)
            selT = [sb(f"selT{j}", [128, 3, TT], F32) for j in range(2)]
            Pm = [sb(f"Pm{j}", [128, NQ, 128], BF16) for j in range(2)]
            i1i = sb("i1i", [128, TT], I32)
            i1a = sb("i1a", [128, TT], I32)
            i1af = sb("i1af", [128, 2, TT], F32)
            NP = 32
            Pa = sb("Pa", [128, NP, 8], BF16)
            Pb = sb("Pb", [128, NP, 16], BF16)
            Qm = [sb(f"Qm{j}", [128, NQ, 128], BF16) for j in range(2)]
            Wall = sb("Wall", [128, TT, 128], BF16)
            UW = [sb(f"UW{j}", [128, 8, G * 128], BF16) for j in range(3)]
            GV = 2
            VW = [sb(f"VW{j}", [128, GV, D], BF16) for j in range(2)]
            ga = [sb(f"ga{j}", [128, TT], BF16) for j in range(2)]
            wa = [sb(f"wa{j}", [128, TT], BF16) for j in range(2)]
            xd = sb("xd", [128, D], F32)
            tD = sb("tD", [128, D], F32)
            std = sb("std", [128, 8], F32)
            B = [pst(f"B{j}", [128, 512]) for j in range(7)]
            BT = pst("BT", [128, 8, 128], BF16)
            Bk = [f"B{j}" for j in range(7)]
            print("phase_b sbuf remaining", nc.sbuf_bytes_remaining)
            FB = 0
            AB = (1, 2)
            OB = 3

            self.dma("sp", kT[:], self.kT16[l].rearrange("p (g n) -> p g n", n=128), self.ck(("kT16", l)), ["kT"])
            self.dma("sp", lng[:], self.ln2_g[l].partition_broadcast(128), [], ["lng"])
            self.dma("sp", lnb[:], self.ln2_b[l].partition_broadcast(128), [], ["lnb"])
            iota16 = self.cst[:, C_IOTA16:C_IOTA16 + 16]
            iota128b = self.cst[:, C_IOTA:C_IOTA + 128]
            tiles = [(b, i) for b in range(self.nseq) for i in range(NT)]
            state = {"wcount": 0, "wqc": 0, "vcount": 0}

            def F1(n):
                b, i = tiles[n]
                par = n % 2
                hTp, hk = hT[par], f"hT{par}"
                sTp, sk = selT[par], f"selT{par}"
                for blk in range(TT // 128):
                    t0 = b * S + i * TT + blk * 128
                    bs = slice(blk * 128, (blk + 1) * 128)
                    self.dma("sp", xt[:], xsrc[t0:t0 + 128, :], [xsrc_key], ["xt"])
                    yield
                    self.ln_stats(xt[:], st, tB[:], ["xt"], "b")
                    yield
                    self.act(tB[:], xt[:], AF.Identity, ["xt", "bst"], ["tB"], bias=st[:, 6:7], scale=st[:, 5:6])
                    self.tt("dve", tB[:], tB[:], scp[:], ALU.mult, ["tB", "scp"], ["tB"])
                    self.tt("dve", hb[:], tB[:], shp[:], ALU.add, ["tB", "shp"], ["hb"])
                    yield
                    yield
                    yield
                    for k in range(8):
                        self.tr(BT[:, k, :], hb[:, k * 128:(k + 1) * 128], self.ident16, ["hb", "cb16"], ["BT"])
                    yield
                    self.cp("act", hTp[:, :, bs], BT[:], ["BT"], [hk])
                    yield
                    yield
                    def wq_load(cg):
                        wc = state["wqc"]
                        state["wqc"] += 1
                        self.dma("sp", wqs[wc % 2][:], self.wq16[l, :, cg * 256:(cg + 1) * 256].rearrange("(k p) c -> p k c", p=128),
                                 self.ck(("wq16", l)), [f"wqs{wc % 2}"])
                        return wqs[wc % 2], f"wqs{wc % 2}"
                    nxt_w = wq_load(0)
                    yield
                    for cg in range(8):
                        wqb, wqk = nxt_w
                        if cg + 1 < 8:
                            nxt_w = wq_load(cg + 1)
                        yield
                        for j in range(2):
                            sl_ = (cg % 2) * 2 + j
                            for k in range(8):
                                self.mm(B[FB][:, sl_ * 128:(sl_ + 1) * 128], wqb[:, k, j * 128:(j + 1) * 128], hTp[:, k, bs],
                                        k == 0, k == 7, [wqk, hk], [Bk[FB]])
                        yield
                        if cg % 2 == 1:
                            c4 = cg // 2
                            self.cp("act", qT[:, c4 * 4:(c4 + 1) * 4, :], B[FB][:, :].rearrange("p (j t) -> p j t", t=128),
                                    [Bk[FB]], ["qT"])
                            yield
                    yield
                    for bq in range(4):
                        for j in range(4):
                            gidx = bq * 4 + j
                            half, h = gidx // 8, gidx % 8
                            self.mm(B[FB][:, j * 128:(j + 1) * 128], qT[:, 2 * h + half, :], kT[:, gidx, :],
                                    True, True, ["qT", "kT"], [Bk[FB]])
                        yield
                        self.cp("act", s12[:, bq * 4:(bq + 1) * 4, :], B[FB][:, :].rearrange("p (j n) -> p j n", n=128),
                                [Bk[FB]], ["s12"])
                        yield
                    yield
                    for g0 in (0, 8):
                        gs = range(g0, g0 + 8)
                        for g in gs:
                            self.P.op("dve", (lambda g=g: lambda e: e.max(out=v12[:, g, 0:8], in_=s12[:, g, :]))(), ["s12"], [("v12", g)])
                        for g in gs:
                            self.P.op("dve", (lambda g=g: lambda e: e.max_index(out=i12[:, g, 0:8], in_max=v12[:, g, 0:8], in_values=s12[:, g, :]))(),
                                      ["s12", ("v12", g)], [("i12", g)])
                        yield
                        for g in gs:
                            self.P.op("dve", (lambda g=g: lambda e: e.match_replace(out=mr[:, g % 8, :], in_to_replace=v12[:, g, 0:8], in_values=s12[:, g, :], imm_value=-1e30))(),
                                      ["s12", ("v12", g)], [("mr", g % 8)])
                        yield
                        for g in gs:
                            self.P.op("dve", (lambda g=g: lambda e: e.max(out=v12[:, g, 8:16], in_=mr[:, g % 8, :]))(), [("mr", g % 8)], [("v12b", g)])
                        for g in gs:
                            self.P.op("dve", (lambda g=g: lambda e: e.max_index(out=i12[:, g, 8:16], in_max=v12[:, g, 8:16], in_values=mr[:, g % 8, :]))(),
                                      [("mr", g % 8), ("v12b", g)], [("i12b", g)])
                        yield
                    vkeys = [("v12", g) for g in range(16)] + [("v12b", g) for g in range(16)]
                    ikeys = [("i12", g) for g in range(16)] + [("i12b", g) for g in range(16)]
                    self.cp("dve", i12f[:], i12[:], ikeys, ["i12f"])
                    self.tt("dve", cand.rearrange("p h (a b) -> p h a b", b=16),
                            v12[:, 0:8, :].unsqueeze(3).to_broadcast([128, 8, 16, 16]),
                            v12[:, 8:16, :].unsqueeze(2).to_broadcast([128, 8, 16, 16]), ALU.add, vkeys, ["s12"])
                    yield
                    for h0 in (0, 2, 4, 6):
                        hs = range(h0, h0 + 2)
                        for h in hs:
                            self.P.op("dve", (lambda h=h: lambda e: e.max(out=ts_[:, h, 0:8], in_=cand[:, h, :]))(), ["s12"], [("ts", h)])
                        for h in hs:
                            self.P.op("dve", (lambda h=h: lambda e: e.max_index(out=pos[:, h, 0:8], in_max=ts_[:, h, 0:8], in_values=cand[:, h, :]))(),
                                      ["s12", ("ts", h)], [("pos", h)])
                        for h in hs:
                            self.P.op("dve", (lambda h=h: lambda e: e.match_replace(out=mrc[:, h % 2, :], in_to_replace=ts_[:, h, 0:8], in_values=cand[:, h, :], imm_value=-1e30))(),
                                      ["s12", ("ts", h)], [("mrc", h % 2)])
                        yield
                        for h in hs:
                            self.P.op("dve", (lambda h=h: lambda e: e.max(out=ts_[:, h, 8:16], in_=mrc[:, h % 2, :]))(), [("mrc", h % 2)], [("tsb", h)])
                        for h in hs:
                            self.P.op("dve", (lambda h=h: lambda e: e.max_index(out=pos[:, h, 8:16], in_max=ts_[:, h, 8:16], in_values=mrc[:, h % 2, :]))(),
                                      [("mrc", h % 2), ("tsb", h)], [("posb", h)])
                        yield
                    tkeys = [("ts", h) for h in range(8)] + [("tsb", h) for h in range(8)]
                    pkeys = [("pos", h) for h in range(8)] + [("posb", h) for h in range(8)]
                    self.ts("dve", pa[:], pos[:], 4, None, ALU.logical_shift_right, None, pkeys, ["pa"])
                    self.ts("dve", pb[:], pos[:], 15, None, ALU.bitwise_and, None, pkeys, ["pb"])
                    self.cp("dve", paf[:].rearrange("p (h k) -> p h k", k=16), pa[:], ["pa"], ["paf"])
                    self.cp("dve", pbf[:].rearrange("p (h k) -> p h k", k=16), pb[:], ["pb"], ["pbf"])
                    yield
                    for which, pf, pk in ((0, paf, "paf"), (1, pbf, "pbf")):
                        for hh in range(4):
                            hs = slice(hh * 32, (hh + 1) * 32)
                            self.tt("dve", oh[:], pf[:, hs].unsqueeze(2).to_broadcast([128, 32, 16]),
                                    iota16.unsqueeze(1).to_broadcast([128, 32, 16]), ALU.is_equal, [pk, "cst"], ["oh"])
                            self.tt("dve", oh[:].rearrange("p (h k) a -> p h k a", k=16), oh[:].rearrange("p (h k) a -> p h k a", k=16),
                                    i12f[:, which * 8 + hh * 2:which * 8 + hh * 2 + 2, :].unsqueeze(2).to_broadcast([128, 2, 16, 16]), ALU.mult,
                                    ["oh", "i12f"], ["oh"])
                            yield
                            self.red(sel[:, which, hs], oh[:], ["oh"], ["sel"])
                            yield
                    tv = ts_[:]
                    g3 = sel[:, 2, :].rearrange("p (h k) -> p h k", k=16)
                    self.tt("dve", g3, tv, tv[:, :, 0:1].to_broadcast([128, 8, 16]), ALU.subtract, tkeys, ["sel"])
                    self.act(sel[:, 2, :], sel[:, 2, :], AF.Exp, ["sel"], ["sel"])
                    yield
                    self.red(ssum[:], g3, ["sel"], ["ssum"])
                    self.P.op("dve", lambda e: e.reciprocal(out=ssum[:], in_=ssum[:]), ["ssum"], ["ssum"])
                    self.tt("dve", g3, g3, ssum[:].unsqueeze(2).to_broadcast([128, 8, 16]), ALU.mult, ["sel", "ssum"], ["sel"])
                    yield
                    yield
                    for j in range(3):
                        self.tr(B[FB][:, j * 128:(j + 1) * 128], sel[:, j, :], self.identf, ["sel", "cst"], [Bk[FB]])
                    yield
                    self.cp("act", sTp[:, :, bs], B[FB][:, 0:384].rearrange("p (j t) -> p j t", t=128), [Bk[FB]], [sk])
                    yield

            def F2D(n, gen):
                b, i = tiles[n]
                par = n % 2
                hTp, hk = hT[par], f"hT{par}"
                sTp, sk = selT[par], f"selT{par}"

                def pull(k=1):
                    if gen is None:
                        return
                    for _ in range(k):
                        try:
                            next(gen)
                        except StopIteration:
                            return
                ib = iota128b.unsqueeze(1).to_broadcast([128, NQ, 128])
                wbanks = (FB, AB[0], AB[1])
                rr = 0
                self.cp("dve", i1i[:], sTp[:, 0, :], [sk], ["i1i"])
                self.ts("dve", i1a[:], i1i[:], 4, None, ALU.logical_shift_right, None, ["i1i"], ["i1a"])
                self.cp("dve", i1af[:, 0, :], i1a[:], ["i1a"], ["i1af"])
                self.ts("dve", i1a[:], i1i[:], 15, None, ALU.bitwise_and, None, ["i1i"], ["i1a"])
                self.cp("dve", i1af[:, 1, :], i1a[:], ["i1a"], ["i1af"])
                for qd in range(TT // NQ):
                    tq = slice(qd * NQ, (qd + 1) * NQ)
                    pm, qm = Pm[qd % 2], Qm[qd % 2]
                    pmk, qmk = f"Pm{qd % 2}", f"Qm{qd % 2}"
                    if (qd * NQ) % NP == 0:
                        tp_ = slice(qd * NQ, qd * NQ + NP)
                        self.tt("dve", Pa[:], iota16[:, 0:8].unsqueeze(1).to_broadcast([128, NP, 8]),
                                i1af[:, 0, tp_].unsqueeze(2).to_broadcast([128, NP, 8]), ALU.is_equal, ["cst", "i1af"], ["Pa"])
                        self.tt("dve", Pa[:], Pa[:], sTp[:, 2, tp_].unsqueeze(2).to_broadcast([128, NP, 8]), ALU.mult,
                                ["Pa", sk], ["Pa"])
                        self.tt("dve", Pb[:], iota16.unsqueeze(1).to_broadcast([128, NP, 16]),
                                i1af[:, 1, tp_].unsqueeze(2).to_broadcast([128, NP, 16]), ALU.is_equal, ["cst", "i1af"], ["Pb"])
                    o_ = (qd * NQ) % NP
                    self.tt("dve", qm[:], ib, sTp[:, 1, tq].unsqueeze(2).to_broadcast([128, NQ, 128]), ALU.is_equal,
                            ["cst", sk], [qmk])
                    self.tt("pool", pm[:].rearrange("p t (a b) -> p t a b", b=16),
                            Pa[:, o_:o_ + NQ, :].unsqueeze(3).to_broadcast([128, NQ, 8, 16]),
                            Pb[:, o_:o_ + NQ, :].unsqueeze(2).to_broadcast([128, NQ, 8, 16]), ALU.mult, ["Pa", "Pb"], [pmk])
                    for t4 in range(NQ // 4):
                        bank = wbanks[rr % 3]
                        rr += 1
                        for j in range(4):
                            tl = t4 * 4 + j
                            self.mm(B[bank][:, j * 128:(j + 1) * 128], qm[:, tl, :], pm[:, tl, :], True, True,
                                    [qmk, pmk], [Bk[bank]])
                        tg = qd * NQ + t4 * 4
                        self.cp("act", Wall[:, tg:tg + 4, :], B[bank][:, :].rearrange("p (j i) -> p j i", i=128),
                                [Bk[bank]], ["Wall"])
                bufs = {}

                def emit_u(ch):
                    g, cc = ch // G, ch % G
                    if cc == 0:
                        wc = state["wcount"]
                        state["wcount"] += 1
                        ub, uk = UW[wc % 3], f"UW{wc % 3}"
                        self.dma("sp", ub[:], self.uG[l, g], [("uG", l, g)], [uk])
                        bufs[("u", g)] = (ub, uk)
                    if ch % GV == 0:
                        vc = state["vcount"]
                        state["vcount"] += 1
                        vb, vk = VW[vc % 2], f"VW{vc % 2}"
                        gv = ch // GV
                        self.dma("sp", vb[:], self.vG[l, gv], [("vG", l, gv)], [vk])
                        bufs[("v", gv)] = (vb, vk)
                    ub, uk = bufs[("u", g)]
                    ab_ = AB[ch % 2]
                    gb, wb = ga[ch % 2], wa[ch % 2]
                    gk, wk_ = f"ga{ch % 2}", f"wa{ch % 2}"
                    for k in range(8):
                        self.mm(B[ab_][:, 0:TT], ub[:, k, cc * 128:(cc + 1) * 128], hTp[:, k, :], k == 0, k == 7,
                                [uk, hk], [Bk[ab_]])
                    self.act(gb[:], B[ab_][:, 0:TT], AF.Gelu, [Bk[ab_]], [gk])
                    self.tt("pool", wb[:], gb[:], Wall[:, :, ch], ALU.mult, [gk, "Wall"], [wk_])

                def emit_v(ch):
                    vb, vk = bufs[("v", ch // GV)]
                    cc = ch % GV
                    wb, wk_ = wa[ch % 2], f"wa{ch % 2}"
                    for sbk in range(TT // 128):
                        for hf in range(2):
                            ob = OB + sbk * 2 + hf
                            self.mm(B[ob][:, :], wb[:, sbk * 128:(sbk + 1) * 128], vb[:, cc, hf * 512:(hf + 1) * 512],
                                    ch == 0, ch == 127, [wk_, vk], [Bk[ob]])

                SKEW = 1
                for ch in range(128 + SKEW):
                    if ch < 128:
                        emit_u(ch)
                    if ch >= SKEW:
                        emit_v(ch - SKEW)
                    if ch >= 3:
                        pull(2)
                pull(10000)
                for sbk in range(TT // 128):
                    t0 = b * S + i * TT + sbk * 128
                    self.dma("sp", xd[:], xsrc[t0:t0 + 128, :], [xsrc_key], ["xd"])
                    for hf in range(2):
                        cs = slice(hf * 512, (hf + 1) * 512)
                        ob = OB + sbk * 2 + hf
                        self.tt("dve", tD[:, cs], B[ob][:, :], gp[:, cs], ALU.mult, [Bk[ob], "gp"], ["tD"])
                    self.stt(xd[:], xd[:], ALPHA, tD[:], ALU.mult, ALU.add, ["xd", "tD"], ["xd"])
                    self.ln_stats(xd[:], std, tD[:], ["xd"], "d")
                    self.act(tD[:], xd[:], AF.Identity, ["xd", "dst"], ["tD"], bias=std[:, 6:7], scale=std[:, 5:6])
                    self.tt("dve", tD[:], tD[:], lng[:], ALU.mult, ["tD", "lng"], ["tD"])
                    self.tt("pool", tD[:], tD[:], lnb[:], ALU.add, ["tD", "lnb"], ["tD"])
                    self.dma("sp", xdst[t0:t0 + 128, :], tD[:], ["tD"], [xdst_key])

            def load_seq(b):
                self.dma("sp", shp[:], self.modd[l, b, 3 * D:4 * D].partition_broadcast(128), [("modd", l)], ["shp"])
                self.dma("sp", scp[:], self.modd[l, b, 4 * D:5 * D].partition_broadcast(128), [("modd", l)], ["scp"])
                self.dma("sp", gp[:], self.modd[l, b, 5 * D:6 * D].partition_broadcast(128), [("modd", l)], ["gp"])

            load_seq(0)
            for _ in F1(0):
                pass
            for n in range(len(tiles)):
                nxt = None
                if n + 1 < len(tiles) and tiles[n + 1][0] == tiles[n][0]:
                    nxt = F1(n + 1)
                F2D(n, nxt)
                if nxt is None and n + 1 < len(tiles):
                    load_seq(tiles[n + 1][0])
                    for _ in F1(n + 1):
                        pass

    def build(self):
        self.declare()
        with ExitStack() as es:
            self.load_consts(es)
            self.plan_casts()
            cur, curk = self.x, "x"
            for l in range(self.layers):
                self.flush_casts(("adaw16", l), ("win16", l), ("wuq16", l), ("wo16", l), ("wq16", l), ("kT16", l))
                self.P.barrier()
                self.mod_phase(l)
                self.P.barrier()
                last = (l == self.layers - 1)
                if self.stop_after == ("a", l):
                    self.phase_a(l, cur, curk, self.out, "out")
                    break
                self.bg = self.bg_stream(l)
                self.phase_a(l, cur, curk, self.x1, "x1")
                self.pump_bg(100000)
                self.P.barrier()
                dst, dstk = (self.out, "out") if last else (self.x2, "x2")
                self.phase_b(l, self.x1, "x1", dst, dstk)
                cur, curk = dst, dstk
            outs = [o for o in reversed(self.P.ops) if o.is_dma]
            self.P.op("sp", None, extra_deps=outs)
            self.P.emit(self.nc)
        return self.nc


def host_inputs(inputs, nseq, core, S):
    f = lambda a: np.ascontiguousarray(np.asarray(a))
    b0 = core * nseq
    m = {}
    m["x"] = f(inputs["x"][b0:b0 + nseq]).reshape(nseq * S, D)
    m["c"] = f(inputs["c"][b0:b0 + nseq])
    m["pos"] = f(inputs["positions"][b0:b0 + nseq]).astype(np.int32)
    return m


_SHARED = {}


def shared_inputs(inputs):
    f = lambda a: np.ascontiguousarray(np.asarray(a, dtype=np.float32))
    m = {}
    for k in ("ada_w", "ada_b", "w_in", "q_norm_g", "kv_norm_g", "w_uq", "conv_w", "w_o", "ln1_g", "ln1_b",
              "peer_wq", "peer_v", "ln2_g", "ln2_b"):
        m[k] = f(inputs[k])
    wukv = np.asarray(inputs["w_ukv"], dtype=np.float32).reshape(L, 128, 8, 128)
    m["w_ukv_k"] = f(wukv[:, :, :, 0:64].reshape(L, 128, 512))
    m["w_ukv_v"] = f(wukv[:, :, :, 64:128].reshape(L, 128, 512))
    k1 = np.asarray(inputs["peer_k1"], dtype=np.float32)
    k2 = np.asarray(inputs["peer_k2"], dtype=np.float32)
    kk = np.stack([k1, k2], axis=1).reshape(L, 16, 128, 128)
    m["peer_kT"] = f(kk.transpose(0, 3, 1, 2).reshape(L, 128, 2048))
    m["peer_uT"] = f(np.asarray(inputs["peer_u"], dtype=np.float32).transpose(0, 2, 1))
    m["consts"] = make_consts()
    return m


def kernel(**inputs):
    x = np.asarray(inputs["x"])
    Bt, S, _ = x.shape
    ncores = 8
    nseq = Bt // ncores
    kb = K(nseq, S)
    nc = kb.build()
    sh = shared_inputs(inputs)
    in_maps = []
    for core in range(ncores):
        m = dict(sh)
        m.update(host_inputs(inputs, nseq, core, S))
        in_maps.append(m)
    res = run_bass_kernel_spmd(nc, in_maps, core_ids=list(range(ncores)))
    out = np.concatenate([np.asarray(r["out"]).reshape(nseq, S, D) for r in res.results], axis=0)
    return out.astype(np.float32)
```

```python
from contextlib import ExitStack

import numpy as np
import concourse.bass as bass
import concourse.mybir as mybir
from concourse.bass_utils import run_bass_kernel_spmd

F32 = mybir.dt.float32
BF16 = mybir.dt.bfloat16
I32 = mybir.dt.int32
U32 = mybir.dt.uint32
ALU = mybir.AluOpType
AF = mybir.ActivationFunctionType
AX = mybir.AxisListType

L = 2
D = 1024
ALPHA = float((2.0 * L) ** 0.25)
LN_EPS = 1e-5
RMS_EPS = 1e-6
NE = 16384

ENGS = ("pe", "act", "dve", "pool", "sp")
EPOCH = 24000
NDS = 40
NDS_SP = 26


class Op:
    __slots__ = ("eng", "fn", "waits", "idx", "needs_inc", "semval", "epoch",
                 "is_dma", "slot", "target", "snap", "line")

    def __init__(self, eng, fn, is_dma=False):
        self.eng = eng
        self.fn = fn
        self.waits = []
        self.idx = 0
        self.needs_inc = False
        self.semval = 0
        self.epoch = 0
        self.is_dma = is_dma
        self.slot = -1
        self.target = 0
        self.snap = None


class _Res:
    __slots__ = ("w", "readers", "dreaders")

    def __init__(self):
        self.w = None
        self.readers = {}
        self.dreaders = []


class Prog:
    def __init__(self):
        self.ops = []
        self.count = {e: 0 for e in ENGS}
        self.res = {}
        self.known = {e: {} for e in ENGS}
        self.known_shared = {e: False for e in ENGS}
        self.slot_last = [None] * NDS
        self.slot_target = [0] * NDS
        self.next_slot = {}

    def _merge(self, eng, other, extra_stream, extra_val):
        k = self.known[eng]
        if self.known_shared[eng]:
            k = dict(k)
            self.known[eng] = k
            self.known_shared[eng] = False
        for s, v in other.items():
            if k.get(s, 0) < v:
                k[s] = v
        if k.get(extra_stream, 0) < extra_val:
            k[extra_stream] = extra_val

    def _add_wait(self, o, d):
        eng = o.eng
        k = self.known[eng]
        if d.is_dma:
            if k.get(("d", d.slot), 0) >= d.target:
                return
            self._merge(eng, d.snap, ("d", d.slot), d.target)
        else:
            if k.get(d.eng, 0) >= d.idx:
                return
            self._merge(eng, d.snap, d.eng, d.idx)
        d.needs_inc = True
        o.waits.append(d)

    def _rs(self, key):
        r = self.res.get(key)
        if r is None:
            r = _Res()
            self.res[key] = r
        return r

    def op(self, eng, fn, reads=(), writes=(), dma=False, extra_deps=()):
        o = Op(eng, fn, is_dma=dma)
        if getattr(self, "trace_lines", False):
            import sys as _s
            fr = _s._getframe(1)
            while fr.f_code.co_name in ("op", "mm", "tr", "act", "tt", "ts", "stt", "cp", "red", "memset", "dma", "proj", "ln_stats"):
                fr = fr.f_back
            o.line = fr.f_lineno
        self.count[eng] += 1
        o.idx = self.count[eng]
        raw = []
        other = []
        ex = [k for k in reads if isinstance(k, str) and (k[0] == "B" or k == "mp")]
        if ex:
            reads = [k for k in reads if k not in ex]
            writes = list(writes) + ex
        for key in reads:
            r = self._rs(key)
            if r.w is not None:
                raw.append(r.w)
        for key in writes:
            r = self._rs(key)
            if r.w is not None:
                other.append(r.w)
            other.extend(r.readers.values())
            other.extend(r.dreaders)
        raw.extend(extra_deps)
        if dma:
            lo, hi = (0, NDS_SP) if eng == "sp" else (NDS_SP, NDS)
            s = self.next_slot.get(eng, lo)
            self.next_slot[eng] = lo + (s + 1 - lo) % (hi - lo)
            o.slot = s
            if self.slot_last[s] is not None:
                raw.append(self.slot_last[s])
            self.slot_target[s] += 16
            o.target = self.slot_target[s]
            self.slot_last[s] = o
        seen = set()
        for d in raw:
            if id(d) in seen or d is o:
                continue
            seen.add(id(d))
            if (not d.is_dma) and d.eng == eng:
                if eng == "pe":
                    continue
                if self.known[eng].get(("self", eng), 0) >= d.idx:
                    continue
                d.needs_inc = True
                o.waits.append(d)
                self._merge(eng, {}, ("self", eng), d.idx)
            else:
                self._add_wait(o, d)
        for d in other:
            if id(d) in seen or d is o:
                continue
            seen.add(id(d))
            if (not d.is_dma) and d.eng == eng:
                if eng != "pool":
                    continue
                if self.known[eng].get(("self", eng), 0) >= d.idx:
                    continue
                d.needs_inc = True
                o.waits.append(d)
                self._merge(eng, {}, ("self", eng), d.idx)
                continue
            self._add_wait(o, d)
        self.known_shared[eng] = True
        o.snap = self.known[eng]
        for key in reads:
            r = self._rs(key)
            if dma:
                r.dreaders.append(o)
            else:
                r.readers[eng] = o
        for key in writes:
            r = self._rs(key)
            r.w = o
            r.readers = {}
            r.dreaders = []
        self.ops.append(o)
        return o

    def barrier(self):
        last = {}
        for o in self.ops:
            if not o.is_dma and o.fn is not None:
                last[o.eng] = o
        deps = list(last.values()) + [d for d in self.slot_last if d is not None]
        for e in ENGS:
            self.op(e, None, extra_deps=deps)

    def emit(self, nc):
        n_epochs = {}
        per = {e: [] for e in ENGS}
        for o in self.ops:
            per[o.eng].append(o)
        for e in ENGS:
            cnt = 0
            for o in per[e]:
                if o.is_dma:
                    continue
                if o.needs_inc:
                    cnt += 1
                    o.epoch = (cnt - 1) // EPOCH
                    o.semval = (cnt - 1) % EPOCH + 1
            n_epochs[e] = (cnt + EPOCH - 1) // EPOCH
        with ExitStack() as es:
            sems = {}
            for e in ENGS:
                for ep in range(n_epochs[e]):
                    sems[(e, ep)] = es.enter_context(nc.semaphore(f"s_{e}_{ep}"))
            dsems = [es.enter_context(nc.semaphore(f"d_{i}")) for i in range(NDS)]
            block = es.enter_context(nc.Block())

            max_ops = getattr(self, "max_ops", None)
            if max_ops is not None:
                keep = set(id(o) for o in self.ops[:max_ops])
                per = {e: [o for o in per[e] if id(o) in keep] for e in ENGS}

            def run(engname):
                def body(e):
                    for o in per[engname]:
                        for d in o.waits:
                            if d.is_dma:
                                e.wait_ge(dsems[d.slot], d.target)
                            else:
                                e.wait_ge(sems[(d.eng, d.epoch)], d.semval)
                        if o.fn is None:
                            continue
                        ins = o.fn(e)
                        if o.is_dma:
                            ins.then_inc(dsems[o.slot], 16)
                        elif o.needs_inc:
                            ins.then_inc(sems[(engname, o.epoch)], 1)
                return body

            block.tensor(run("pe"))
            block.scalar(run("act"))
            block.vector(run("dve"))
            block.gpsimd(run("pool"))
            block.sync(run("sp"))


C_ID = 0
C_IOTA = 128
C_TRI = 256
C_MASK = 384
C_ONES = 512
C_IOTA16 = 640
C_INVF = 656
C_ONE1 = 657
NCONST = 658


def make_consts():
    c = np.zeros((128, NCONST), np.float32)
    c[:, C_ID:C_ID + 128] = np.eye(128)
    c[:, C_IOTA:C_IOTA + 128] = np.arange(128)[None, :]
    j = np.arange(128)[:, None]
    k = np.arange(128)[None, :]
    c[:, C_TRI:C_TRI + 128] = (j > k)
    c[:, C_MASK:C_MASK + 128] = (j < k)
    c[:, C_ONES:C_ONES + 128] = 1.0
    c[:, C_IOTA16:C_IOTA16 + 16] = np.arange(16)[None, :]
    inv = (10000.0 ** (-np.arange(16, dtype=np.float32) / 16.0)).astype(np.float32)
    for p in range(64, 96):
        c[p, C_INVF] = inv[(p - 64) % 16]
    c[:64, C_ONE1] = 1.0
    return c


class K:
    def __init__(self, nseq, S, layers=L, stop_after=None, dbg=False):
        self.nseq, self.S = nseq, S
        self.T = nseq * S
        self.NB = S // 128
        self.layers = layers
        self.stop_after = stop_after
        self.nc = bass.Bass("TRN2", target_bir_lowering=False)
        self.P = Prog()
        self.dbg = dbg
        self.dbg_out = {}

    def mm(self, out, lhsT, rhs, start, stop, R, W):
        return self.P.op("pe", lambda e: e.matmul(out, lhsT, rhs, start=start, stop=stop), R, W)

    def tr(self, out, in_, ident, R, W):
        return self.P.op("pe", lambda e: e.transpose(out=out, in_=in_, identity=ident), R, W)

    def act(self, out, in_, func, R, W, bias=None, scale=1.0):
        if bias is None:
            return self.P.op("act", lambda e: e.activation(out=out, in_=in_, func=func, scale=scale), R, W)
        return self.P.op("act", lambda e: e.activation(out=out, in_=in_, func=func, bias=bias, scale=scale), R, W)

    def tt(self, eng, out, in0, in1, op, R, W):
        return self.P.op(eng, lambda e: e.tensor_tensor(out=out, in0=in0, in1=in1, op=op), R, W)

    def ts(self, eng, out, in0, s1, s2, op0, op1, R, W):
        if s2 is None:
            return self.P.op(eng, lambda e: e.tensor_scalar(out=out, in0=in0, scalar1=s1, scalar2=None, op0=op0), R, W)
        return self.P.op(eng, lambda e: e.tensor_scalar(out=out, in0=in0, scalar1=s1, scalar2=s2, op0=op0, op1=op1), R, W)

    def stt(self, out, in0, scalar, in1, op0, op1, R, W):
        return self.P.op("dve", lambda e: e.scalar_tensor_tensor(out=out, in0=in0, scalar=scalar, in1=in1, op0=op0, op1=op1), R, W)

    def cp(self, eng, out, in_, R, W):
        if eng == "act":
            return self.P.op("act", lambda e: e.activation(out=out, in_=in_, func=AF.Copy), R, W)
        return self.P.op(eng, lambda e: e.tensor_copy(out=out, in_=in_), R, W)

    def red(self, out, in_, R, W, op=ALU.add):
        return self.P.op("dve", lambda e: e.tensor_reduce(out=out, in_=in_, axis=AX.X, op=op), R, W)

    def memset(self, eng, ap, val, W):
        return self.P.op(eng, lambda e: e.memset(ap, val), (), W)

    def dma(self, q, out, in_, R, W, slow=False):
        if slow:
            return self.P.op(q, lambda e: e.dma_start(out=out, in_=in_, allow_slow_non_contiguous=True), R, W, dma=True)
        return self.P.op(q, lambda e: e.dma_start(out=out, in_=in_), R, W, dma=True)

    def din(self, name, shape, dt=F32):
        return self.nc.dram_tensor(name, list(shape), dt, kind="ExternalInput").ap()

    def dscr(self, name, shape, dt):
        return self.nc.dram_tensor(name, list(shape), dt).ap()

    def declare(self):
        T, S, nseq = self.T, self.S, self.nseq
        self.x = self.din("x", [T, D])
        self.c = self.din("c", [nseq, D])
        self.pos = self.din("pos", [nseq, S], I32)
        self.consts = self.din("consts", [128, NCONST])
        self.ada_w = self.din("ada_w", [L, D, 6 * D])
        self.ada_b = self.din("ada_b", [L, 6 * D])
        self.w_in = self.din("w_in", [L, D, 1952])
        self.q_norm_g = self.din("q_norm_g", [L, 256])
        self.kv_norm_g = self.din("kv_norm_g", [L, 128])
        self.w_uq = self.din("w_uq", [L, 256, 768])
        self.w_ukv_k = self.din("w_ukv_k", [L, 128, 512])
        self.w_ukv_v = self.din("w_ukv_v", [L, 128, 512])
        self.conv_w = self.din("conv_w", [L, 3, 256])
        self.w_o = self.din("w_o", [L, D, D])
        self.ln1_g = self.din("ln1_g", [L, D])
        self.ln1_b = self.din("ln1_b", [L, D])
        self.peer_wq = self.din("peer_wq", [L, D, 2048])
        self.peer_kT = self.din("peer_kT", [L, 128, 16 * 128])
        self.peer_uT = self.din("peer_uT", [L, D, NE])
        self.peer_v = self.din("peer_v", [L, NE, D])
        self.ln2_g = self.din("ln2_g", [L, D])
        self.ln2_b = self.din("ln2_b", [L, D])
        self.out = self.nc.dram_tensor("out", [T, D], F32, kind="ExternalOutput").ap()
        self.adaw16 = self.dscr("adaw16", [L, D, 6 * D], BF16)
        self.win16 = self.dscr("win16", [L, D, 1952], BF16)
        self.wuq16 = self.dscr("wuq16", [L, 256, 768], BF16)
        self.wo16 = self.dscr("wo16", [L, D, D], BF16)
        self.wq16 = self.dscr("wq16", [L, D, 2048], BF16)
        self.kT16 = self.dscr("kT16", [L, 128, 2048], BF16)
        self.uT16 = self.dscr("uT16", [L, 128, 8, NE], BF16)
        self.v16 = self.dscr("v16", [L, NE, D], BF16)
        self.uG = self.dscr("uG", [L, 32, 128, 8, 512], BF16)
        self.vG = self.dscr("vG", [L, 64, 128, 2, D], BF16)
        self.modd = self.dscr("modd", [L, nseq, 6 * D], F32)
        self.x1 = self.dscr("x1", [T, D], F32)
        self.x2 = self.dscr("x2", [T, D], F32)

    def plan_casts(self):
        self.cast_keys = {}
        self.cast_q = {}

        def add(group, dst, src):
            keys = self.cast_keys.setdefault(group, [])
            key = (group, len(keys))
            keys.append(key)
            self.cast_q.setdefault(group, []).append((dst, src, key))

        for l in range(self.layers):
            def cast2d(group, dst, src, rows, cols):
                cstep = min(cols, 4096)
                for r0 in range(0, rows, 128):
                    for c0 in range(0, cols, cstep):
                        c1 = min(cols, c0 + cstep)
                        add(group, dst[r0:r0 + 128, c0:c1], src[r0:r0 + 128, c0:c1])
            cast2d(("adaw16", l), self.adaw16[l], self.ada_w[l], D, 6 * D)
            cast2d(("win16", l), self.win16[l], self.w_in[l], D, 1952)
            cast2d(("wuq16", l), self.wuq16[l], self.w_uq[l], 256, 768)
            cast2d(("wo16", l), self.wo16[l], self.w_o[l], D, D)
            cast2d(("wq16", l), self.wq16[l], self.peer_wq[l], D, 2048)
            cast2d(("kT16", l), self.kT16[l], self.peer_kT[l], 128, 2048)
            for c0 in range(0, NE, 4096):
                for dk in range(8):
                    add(("uT16", l), self.uT16[l, :, dk, c0:c0 + 4096],
                        self.peer_uT[l, dk * 128:(dk + 1) * 128, c0:c0 + 4096])
            vs = self.peer_v[l].rearrange("(a b) d -> a (b d)", b=4)
            vd = self.v16[l].rearrange("(a b) d -> a (b d)", b=4)
            for r0 in range(0, NE // 4, 128):
                add(("v16", l), vd[r0:r0 + 128, :], vs[r0:r0 + 128, :])

    def flush_casts(self, *groups):
        for g in groups:
            for dst, src, key in self.cast_q.pop(g, []):
                self.dma("pool", dst, src, [], [key])

    def bg_stream(self, l):
        for g in (("uT16", l), ("v16", l)):
            for dst, src, key in self.cast_q.pop(g, []):
                self.dma("pool", dst, src, [], [key])
                yield
        for g in range(32):
            self.dma("sp", self.uG[l, g], self.uT16[l, :, :, g * 512:(g + 1) * 512], self.ck(("uT16", l)), [("uG", l, g)])
            yield
        for gv in range(64):
            self.dma("sp", self.vG[l, gv], self.v16[l, gv * 256:(gv + 1) * 256, :].rearrange("(c p) d -> p c d", p=128),
                     self.ck(("v16", l)), [("vG", l, gv)])
            yield
        if l + 1 < self.layers:
            for g in (("adaw16", l + 1), ("win16", l + 1), ("wuq16", l + 1), ("wo16", l + 1), ("wq16", l + 1), ("kT16", l + 1)):
                for dst, src, key in self.cast_q.pop(g, []):
                    self.dma("pool", dst, src, [], [key])
                    yield

    def pump_bg(self, k):
        g = getattr(self, "bg", None)
        if g is None:
            return
        for _ in range(k):
            try:
                next(g)
            except StopIteration:
                self.bg = None
                return

    def ck(self, group):
        return list(self.cast_keys[group])

    def load_consts(self, es):
        nc = self.nc
        self.cst = es.enter_context(nc.sbuf_tensor("cst", [128, NCONST], F32))
        self.cb16 = es.enter_context(nc.sbuf_tensor("cb16", [128, 640], BF16))
        self.dma("sp", self.cst[:], self.consts, [], ["cst"])
        self.cp("dve", self.cb16[:], self.cst[:, 0:640], ["cst"], ["cb16"])
        self.ident16 = self.cb16[:, C_ID:C_ID + 128]
        self.tri16 = self.cb16[:, C_TRI:C_TRI + 128]
        self.mask16 = self.cb16[:, C_MASK:C_MASK + 128]
        self.ones16 = self.cb16[:, C_ONES:C_ONES + 128]
        self.identf = self.cst[:, C_ID:C_ID + 128]
        self.onesf = self.cst[:, C_ONES:C_ONES + 128]
        self.maskf = self.cst[:, C_MASK:C_MASK + 128]

    def ln_stats(self, src, st, sq, Rsrc, pfx):
        self.red(st[:, 0:1], src, Rsrc, [pfx + "st"])
        self.act(sq, src, AF.Square, Rsrc, [pfx + "sq"])
        self.red(st[:, 1:2], sq, [pfx + "sq"], [pfx + "st"])
        self.ts("dve", st[:, 2:3], st[:, 0:1], 1.0 / D, None, ALU.mult, None, [pfx + "st"], [pfx + "st"])
        self.tt("dve", st[:, 3:4], st[:, 2:3], st[:, 2:3], ALU.mult, [pfx + "st"], [pfx + "st"])
        self.stt(st[:, 4:5], st[:, 1:2], 1.0 / D, st[:, 3:4], ALU.mult, ALU.subtract, [pfx + "st"], [pfx + "st"])
        self.act(st[:, 5:6], st[:, 4:5], AF.Ln, [pfx + "st"], [pfx + "st"], bias=LN_EPS)
        self.act(st[:, 5:6], st[:, 5:6], AF.Exp, [pfx + "st"], [pfx + "st"], scale=-0.5)
        self.stt(st[:, 6:7], st[:, 2:3], -1.0, st[:, 5:6], ALU.mult, ALU.mult, [pfx + "st"], [pfx + "st"])

    def mod_phase(self, l):
        nc, nseq = self.nc, self.nseq
        with ExitStack() as es:
            cT = es.enter_context(nc.sbuf_tensor(f"cT{l}", [128, 8, nseq], F32))
            cTa = es.enter_context(nc.sbuf_tensor(f"cTa{l}", [128, 8, nseq], BF16))
            aw = es.enter_context(nc.sbuf_tensor(f"aw{l}", [128, 8, 6 * D], BF16))
            ab = es.enter_context(nc.sbuf_tensor(f"ab{l}", [nseq, 6 * D], F32))
            mo = es.enter_context(nc.sbuf_tensor(f"mo{l}", [nseq, 6 * D], F32))
            mp = es.enter_context(nc.psum_tensor(f"mp{l}", [nseq, 512], F32))
            for b in range(nseq):
                self.dma("sp", cT[:, :, b], self.c[b].rearrange("(k p) -> p k", p=128), [], ["cT"], slow=True)
                self.dma("sp", ab[b:b + 1, :], self.ada_b[l:l + 1, :], [], ["ab"])
            self.act(cTa[:], cT[:], AF.Silu, ["cT"], ["cTa"])
            for k in range(8):
                self.dma("sp", aw[:, k, :], self.adaw16[l, k * 128:(k + 1) * 128, :], self.ck(("adaw16", l)), ["aw"])
            for g in range(12):
                for k in range(8):
                    self.mm(mp[:], cTa[:, k, :], aw[:, k, g * 512:(g + 1) * 512], k == 0, k == 7,
                            ["cTa", "aw"], ["mp"])
                self.tt("dve", mo[:, g * 512:(g + 1) * 512], mp[:], ab[:, g * 512:(g + 1) * 512], ALU.add,
                        ["mp", "ab"], ["mo"])
            for slot in (1, 2, 4, 5):
                self.ts("dve", mo[:, slot * D:(slot + 1) * D], mo[:, slot * D:(slot + 1) * D], 1.0, None, ALU.add, None,
                        ["mo"], ["mo"])
            self.dma("sp", self.modd[l], mo[:], ["mo"], [("modd", l)])

    def phase_a(self, l, xsrc, xsrc_key, xdst, xdst_key):
        nc, S, NB = self.nc, self.S, self.NB
        with ExitStack() as es:
            sb = lambda n, s, d: es.enter_context(nc.sbuf_tensor(f"{n}_a{l}", s, d))
            pst = lambda n, s, d=F32: es.enter_context(nc.psum_tensor(f"{n}_a{l}", s, d))
            win = sb("win", [128, 8, 1952], BF16)
            wkr = sb("wkr", [128, 8, 128], BF16)
            wkrr = sb("wkrr", [128, 8, 128], BF16)
            wuq = sb("wuq", [128, 2, 800], BF16)
            wuqr = sb("wuqr", [128, 2, 800], BF16)
            wk = sb("wk", [128, 512], BF16)
            wv = sb("wv", [128, 512], BF16)
            wtmp = sb("wtmp", [128, 1024], F32)
            wo = sb("wo", [128, 8, 1024], BF16)
            cw = sb("cw", [128, 2, 3], F32)
            gq = sb("gq", [128, 2], F32)
            gkv = sb("gkv", [128, 1], F32)
            lng = sb("lng", [128, D], F32)
            lnb = sb("lnb", [128, D], F32)
            scp = sb("scp", [128, D], F32)
            shp = sb("shp", [128, D], F32)
            gp = sb("gp", [128, D], F32)
            KT = sb("KT", [128, 8, S], BF16)
            Vaug = sb("Vaug", [128, NB, 8, 65], BF16)
            SKT = sb("SKT", [128, 2, S], BF16)
            SV = sb("SV", [128, NB, 256], BF16)
            uT = sb("uT", [128, 2, 130], F32)
            xt = sb("xt", [128, D], F32)
            tA = sb("tA", [128, D], F32)
            tB = sb("tB", [128, D], F32)
            st = sb("st", [128, 8], F32)
            hb = sb("hb", [128, D], BF16)
            hT = sb("hT", [128, 8, 128], BF16)
            qlat = sb("qlat", [128, 2, 128], BF16)
            sqq = sb("sqq", [128, 2, 128], F32)
            kvlat = sb("kvlat", [128, 128], BF16)
            sqkv = sb("sqkv", [128, 128], F32)
            rqb = sb("rqb", [128, 128], F32)
            rkvb = sb("rkvb", [128, 128], F32)
            rkvt = sb("rkvt", [128, 1], F32)
            posi = sb("posi", [128, 128], I32)
            ang = sb("ang", [128, 128], F32)
            angi = sb("angi", [128, 128], I32)
            angf = sb("angf", [128, 128], F32)
            CS1 = sb("CS1", [128, 128], F32)
            CS2 = sb("CS2", [128, 128], F32)
            q1 = sb("q1", [128, 4, 128], F32)
            q2 = sb("q2", [128, 4, 128], F32)
            QT = sb("QT", [128, 8, 128], BF16)
            krf = sb("krf", [128, 128], F32)
            krf2 = sb("krf2", [128, 128], F32)
            SQT = sb("SQT", [128, 2, 128], BF16)
            ccs = sb("ccs", [128, 2, 128], F32)
            yt = sb("yt", [128, 128], F32)
            PT = [sb(f"PT{j}", [128, 4, 128], BF16) for j in range(2)]
            rec = sb("rec", [128, 4, 1], F32)
            mix = sb("mix", [128, D], BF16)
            mixT = sb("mixT", [128, 8, 128], BF16)
            esb = sb("esb", [128, 4, 128], F32)
            lp = sb("lp", [128, 4, 128], F32)
            L1 = sb("L1", [128, 4, 128], BF16)
            Ls = [sb(f"Ls{j}", [128, 128], BF16) for j in range(2)]
            lg = sb("lg", [128, 4, 128], F32)
            WT = [sb(f"WT{j}", [128, 4, 128], BF16) for j in range(2)]
            B = [pst(f"B{j}", [128, 512]) for j in range(7)]
            BT = pst("BT", [128, 8, 128], BF16)
            Bk = [f"B{j}" for j in range(7)]

            for k in range(8):
                self.dma("sp", win[:, k, :], self.win16[l, k * 128:(k + 1) * 128, :], self.ck(("win16", l)), ["win"])
                self.dma("sp", wo[:, k, :], self.wo16[l, k * 128:(k + 1) * 128, :], self.ck(("wo16", l)), ["wo"])
            self.memset("pool", wuq[:], 0.0, ["wuq"])
            for k in range(2):
                self.dma("sp", wuq[:, k, 0:768], self.wuq16[l, k * 128:(k + 1) * 128, :], self.ck(("wuq16", l)), ["wuq"])
            self.dma("sp", gq[:], self.q_norm_g[l].rearrange("(k p) -> p k", p=128), [], ["gq"], slow=True)
            self.dma("sp", gkv[:], self.kv_norm_g[l].rearrange("(p o) -> p o", o=1), [], ["gkv"], slow=True)
            for tap in range(3):
                self.dma("sp", cw[:, :, tap], self.conv_w[l, tap].rearrange("(k p) -> p k", p=128), [], ["cw"], slow=True)
            self.dma("sp", lng[:], self.ln1_g[l].partition_broadcast(128), [], ["lng"])
            self.dma("sp", lnb[:], self.ln1_b[l].partition_broadcast(128), [], ["lnb"])
            self.memset("pool", wkr[:], 0.0, ["wkr"])
            self.memset("pool", wkrr[:], 0.0, ["wkrr"])
            self.cp("pool", wkr[:, :, 64:96], win[:, :, 384:416], ["win"], ["wkr"])
            self.ts("pool", wkrr[:, :, 64:80], win[:, :, 400:416], -1.0, None, ALU.mult, None, ["win"], ["wkrr"])
            self.cp("pool", wkrr[:, :, 80:96], win[:, :, 384:400], ["win"], ["wkrr"])
            for k in range(2):
                self.ts("dve", wuq[:, k, :], wuq[:, k, :], gq[:, k:k + 1], None, ALU.mult, None, ["wuq", "gq"], ["wuq"])
            self.memset("pool", wuqr[:], 0.0, ["wuqr"])
            wv4 = wuq[:, :, 0:768].rearrange("p k (h c) -> p k h c", c=96)
            wr4 = wuqr[:, :, 0:768].rearrange("p k (h c) -> p k h c", c=96)
            for k in range(2):
                self.ts("pool", wr4[:, k, :, 64:80], wv4[:, k, :, 80:96], -1.0, None, ALU.mult, None, ["wuq"], ["wuqr"])
                self.cp("pool", wr4[:, k, :, 80:96], wv4[:, k, :, 64:80], ["wuq"], ["wuqr"])
            self.dma("sp", wtmp[:, 0:512], self.w_ukv_k[l], [], ["wtmp"])
            self.dma("sp", wtmp[:, 512:1024], self.w_ukv_v[l], [], ["wtmp"])
            self.ts("dve", wk[:], wtmp[:, 0:512], gkv[:, 0:1], None, ALU.mult, None, ["wtmp", "gkv"], ["wk"])
            self.ts("dve", wv[:], wtmp[:, 512:1024], gkv[:, 0:1], None, ALU.mult, None, ["wtmp", "gkv"], ["wv"])

            inv_sqrt96 = float(96.0 ** -0.5)
            self.memset("pool", CS1[0:64, :], 1.0, ["CS1"])
            self.memset("pool", CS1[64:128, :], 0.0, ["CS1"])
            self.memset("pool", CS2[:, :], 0.0, ["CS2"])
            self.memset("pool", KT[64:128, :, :], 0.0, ["KT"])
            for b in range(self.nseq):
                self.dma("sp", shp[:], self.modd[l, b, 0:D].partition_broadcast(128), [("modd", l)], ["shp"])
                self.dma("sp", scp[:], self.modd[l, b, D:2 * D].partition_broadcast(128), [("modd", l)], ["scp"])
                self.dma("sp", gp[:], self.modd[l, b, 2 * D:3 * D].partition_broadcast(128), [("modd", l)], ["gp"])
                self.memset("pool", uT[:], 0.0, ["uT"])
                self.memset("pool", Vaug[:], 1.0, ["Vaug"])
                for i in range(NB):
                    t0 = b * S + i * 128
                    s0 = i * 128
                    sl = slice(s0, s0 + 128)
                    self.dma("sp", xt[:], xsrc[t0:t0 + 128, :], [xsrc_key], ["xt"])
                    self.ln_stats(xt[:], st, tA[:], ["xt"], "a")
                    self.act(tB[:], xt[:], AF.Identity, ["xt", "ast"], ["tB"], bias=st[:, 6:7], scale=st[:, 5:6])
                    self.tt("dve", tB[:], tB[:], scp[:], ALU.mult, ["tB", "scp"], ["tB"])
                    self.tt("dve", hb[:], tB[:], shp[:], ALU.add, ["tB", "shp"], ["hb"])
                    for k in range(8):
                        self.tr(BT[:, k, :], hb[:, k * 128:(k + 1) * 128], self.ident16, ["hb", "cb16"], ["BT"])
                    self.cp("act", hT[:], BT[:], ["BT"], ["hT"])
                    self.dma("sp", posi[64:96, :], self.pos[b, sl].partition_broadcast(32), [], ["posi"])
                    self.cp("dve", ang[64:96, :], posi[64:96, :], ["posi"], ["ang"])
                    self.ts("dve", ang[64:96, :], ang[64:96, :], self.cst[64:96, C_INVF:C_INVF + 1],
                            float(1.0 / (2 * np.pi)), ALU.mult, ALU.mult, ["ang", "cst"], ["ang"])
                    self.cp("dve", angi[64:96, :], ang[64:96, :], ["ang"], ["angi"])
                    self.cp("dve", angf[64:96, :], angi[64:96, :], ["angi"], ["angf"])
                    self.tt("dve", angf[64:96, :], ang[64:96, :], angf[64:96, :], ALU.subtract, ["ang", "angf"], ["angf"])
                    self.act(CS2[64:96, :], angf[64:96, :], AF.Sin, ["angf"], ["CS2"], scale=float(2 * np.pi))
                    self.ts("dve", ang[64:96, :], ang[64:96, :], 0.25, None, ALU.add, None, ["ang"], ["ang"])
                    self.cp("dve", angi[64:96, :], ang[64:96, :], ["ang"], ["angi"])
                    self.cp("dve", angf[64:96, :], angi[64:96, :], ["angi"], ["angf"])
                    self.tt("dve", angf[64:96, :], ang[64:96, :], angf[64:96, :], ALU.subtract, ["ang", "angf"], ["angf"])
                    self.act(CS1[64:96, :], angf[64:96, :], AF.Sin, ["angf"], ["CS1"], scale=float(2 * np.pi))
                    if i == 0 and b == 0:
                        pass

                    def proj(bank, slot, lhs_of_k, M):
                        for k in range(8):
                            self.mm(B[bank][0:M, slot * 128:(slot + 1) * 128], lhs_of_k(k), hT[:, k, :], k == 0, k == 7,
                                    ["hT", "win", "wkr", "wkrr"], [Bk[bank]])
                    wcol = lambda c0, w=128: (lambda k: win[:, k, c0:c0 + w])
                    proj(0, 0, wcol(0), 128)
                    proj(0, 1, wcol(128), 128)
                    proj(0, 2, wcol(256), 128)
                    proj(1, 0, lambda k: wkr[:, k, :], 128)
                    proj(1, 1, lambda k: wkrr[:, k, :], 128)
                    proj(1, 2, wcol(1184), 128)
                    proj(1, 3, wcol(1312), 128)
                    proj(2, 0, wcol(1440), 128)
                    proj(2, 1, wcol(1568), 128)
                    proj(2, 2, wcol(672), 128)
                    proj(2, 3, wcol(800), 128)
                    proj(3, 0, wcol(928), 128)
                    proj(3, 1, wcol(1056), 128)
                    proj(3, 2, wcol(416), 128)
                    proj(3, 3, wcol(544), 128)
                    for k in range(8):
                        self.mm(B[4][:, 0:256], hT[:, k, :], win[:, k, 1696:1952], k == 0, k == 7, ["hT", "win"], [Bk[4]])
                    b0v = B[0][:, 0:256].rearrange("p (c t) -> p c t", t=128)
                    self.cp("act", qlat[:], b0v, [Bk[0]], ["qlat"])
                    self.act(sqq[:], b0v, AF.Square, [Bk[0]], ["sqq"])
                    self.cp("act", kvlat[:], B[0][:, 256:384], [Bk[0]], ["kvlat"])
                    self.act(sqkv[:], B[0][:, 256:384], AF.Square, [Bk[0]], ["sqkv"])
                    self.cp("act", SQT[:], B[1][:, 256:512].rearrange("p (c t) -> p c t", t=128), [Bk[1]], ["SQT"])
                    self.cp("act", SKT[:, :, sl], B[2][:, 0:256].rearrange("p (c t) -> p c t", t=128), [Bk[2]], ["SKT"])
                    self.cp("act", SV[:, i, :], B[4][:, 0:256], [Bk[4]], ["SV"])
                    self.cp("act", ccs[:], B[2][:, 256:512].rearrange("p (c t) -> p c t", t=128), [Bk[2]], ["ccs"])
                    self.tt("dve", krf[:, :], B[1][:, 0:128], CS1[:, :], ALU.mult, [Bk[1], "CS1"], ["krf"])
                    self.tt("dve", krf2[:, :], B[1][:, 128:256], CS2[:, :], ALU.mult, [Bk[1], "CS2"], ["krf2"])
                    self.tt("pool", krf[64:96, :], krf[64:96, :], krf2[64:96, :], ALU.add, ["krf", "krf2"], ["krf"])
                    self.cp("pool", KT[64:96, :, sl], krf[64:96, :].unsqueeze(1).to_broadcast([32, 8, 128]), ["krf"], ["KT"])
                    self.tt("dve", uT[:, :, 2:130], ccs[:], B[3][:, 0:256].rearrange("p (c t) -> p c t", t=128), ALU.mult,
                            ["ccs", Bk[3]], ["uT"])
                    for c in range(2):
                        self.ts("dve", yt[:], uT[:, c, 2:130], cw[:, c, 2:3], None, ALU.mult, None, ["uT", "cw"], ["yt"])
                        self.stt(yt[:], uT[:, c, 1:129], cw[:, c, 1:2], yt[:], ALU.mult, ALU.add, ["uT", "cw", "yt"], ["yt"])
                        self.stt(yt[:], uT[:, c, 0:128], cw[:, c, 0:1], yt[:], ALU.mult, ALU.add, ["uT", "cw", "yt"], ["yt"])
                        self.tt("dve", mixT[:, 4 + c, :], yt[:], B[3][:, 256 + c * 128:384 + c * 128], ALU.mult,
                                ["yt", Bk[3]], ["mixT"])
                    self.cp("pool", uT[:, :, 0:2], uT[:, :, 128:130], ["uT"], ["uT"])
                    for c in range(2):
                        self.mm(B[5][:, 0:128], self.onesf, sqq[:, c, :], c == 0, c == 1, ["cst", "sqq"], [Bk[5]])
                    self.mm(B[5][:, 128:256], self.onesf, sqkv[:], True, True, ["cst", "sqkv"], [Bk[5]])
                    self.mm(B[5][:, 256:257], sqkv[:], self.onesf[:, 0:1], True, True, ["cst", "sqkv"], [Bk[5]])
                    self.act(rqb[:], B[5][:, 0:128], AF.Ln, [Bk[5]], ["rqb"], bias=RMS_EPS, scale=1.0 / 256)
                    self.act(rqb[:], rqb[:], AF.Exp, ["rqb"], ["rqb"], scale=-0.5)
                    self.act(rkvb[:], B[5][:, 128:256], AF.Ln, [Bk[5]], ["rkvb"], bias=RMS_EPS, scale=1.0 / 128)
                    self.act(rkvb[:], rkvb[:], AF.Exp, ["rkvb"], ["rkvb"], scale=-0.5)
                    self.act(rkvt[:], B[5][:, 256:257], AF.Ln, [Bk[5]], ["rkvt"], bias=RMS_EPS, scale=1.0 / 128)
                    self.act(rkvt[:], rkvt[:], AF.Exp, ["rkvt"], ["rkvt"], scale=-0.5)
                    for hg in range(2):
                        for hh in range(4):
                            h = hg * 4 + hh
                            for k in range(2):
                                self.mm(B[0][:, hh * 128:(hh + 1) * 128], wuq[:, k, h * 96:h * 96 + 128], qlat[:, k, :],
                                        k == 0, k == 1, ["wuq", "qlat"], [Bk[0]])
                            for k in range(2):
                                self.mm(B[1][:, hh * 128:(hh + 1) * 128], wuqr[:, k, h * 96:h * 96 + 128], qlat[:, k, :],
                                        k == 0, k == 1, ["wuqr", "qlat"], [Bk[1]])
                        v0 = B[0][:, :].rearrange("p (h t) -> p h t", t=128)
                        v1 = B[1][:, :].rearrange("p (h t) -> p h t", t=128)
                        bc = lambda a: a.unsqueeze(1).to_broadcast([128, 4, 128])
                        self.tt("dve", q1[:], v0, bc(CS1[:, :]), ALU.mult, [Bk[0], "CS1"], ["q1"])
                        self.tt("dve", q2[:], v1, bc(CS2[:, :]), ALU.mult, [Bk[1], "CS2"], ["q2"])
                        self.tt("pool", q1[:], q1[:], q2[:], ALU.add, ["q1", "q2"], ["q1"])
                        self.tt("pool", QT[:, hg * 4:(hg + 1) * 4, :], q1[:], bc(rqb[:, :]), ALU.mult,
                                ["q1", "rqb"], ["QT"])
                    for hg in range(2):
                        for hh in range(4):
                            h = hg * 4 + hh
                            self.mm(B[2][0:64, hh * 128:(hh + 1) * 128], wk[:, h * 64:(h + 1) * 64], kvlat[:], True, True,
                                    ["wk", "kvlat"], [Bk[2]])
                        self.tt("dve", KT[0:64, hg * 4:(hg + 1) * 4, sl], B[2][0:64, :].rearrange("p (h t) -> p h t", t=128),
                                rkvb[0:64, :].unsqueeze(1).to_broadcast([64, 4, 128]), ALU.mult, [Bk[2], "rkvb"], ["KT"])
                    self.mm(B[3][:, :], kvlat[:], wv[:], True, True, ["kvlat", "wv"], [Bk[3]])
                    self.act(Vaug[:, i, :, 0:64], B[3][:, :].rearrange("p (h c) -> p h c", c=64), AF.Identity, [Bk[3], "rkvt"],
                             ["Vaug"], scale=rkvt[:, 0:1])

                    nk = i + 1

                    def mla_gen():
                        gm = 0
                        for h in range(8):
                            ob = 5 + (h // 4)
                            hh = h % 4
                            ov = B[ob][:, 0:260].rearrange("p (h c) -> p h c", c=65)
                            for g0 in range(0, nk, 4):
                                kbs = list(range(g0, min(g0 + 4, nk)))
                                sbk = 3 + (gm % 2)
                                pt = PT[gm % 2]
                                ptk = f"PT{gm % 2}"
                                gm += 1
                                for j, kb in enumerate(kbs):
                                    self.mm(B[sbk][:, j * 128:(j + 1) * 128], KT[:, h, kb * 128:(kb + 1) * 128], QT[:, h, :],
                                            True, True, ["KT", "QT"], [Bk[sbk]])
                                n = len(kbs)
                                self.act(pt[:, 0:n, :], B[sbk][:, 0:n * 128].rearrange("p (j t) -> p j t", t=128), AF.Exp,
                                         [Bk[sbk]], [ptk], scale=inv_sqrt96)
                                if kbs[-1] == i:
                                    self.memset("pool", pt[64:128, n - 1, 0:64], 0.0, [ptk])
                                yield
                                for j, kb in enumerate(kbs):
                                    self.mm(ov[:, hh, :], pt[:, j, :], Vaug[:, kb, h, :], kb == 0, kb == i,
                                            [ptk, "Vaug"], [Bk[ob]])
                                yield
                            if hh == 3:
                                self.P.op("dve", (lambda ov=ov: lambda e: e.reciprocal(out=rec[:], in_=ov[:, :, 64:65]))(),
                                          [Bk[ob]], ["rec"])
                                self.tt("dve", mix[:, (h // 4) * 256:(h // 4 + 1) * 256].rearrange("p (h c) -> p h c", c=64),
                                        ov[:, :, 0:64], rec[:].to_broadcast([128, 4, 64]), ALU.mult, [Bk[ob], "rec"], ["mix"])
                                yield

                    def sb_gen():
                        lsi = 0
                        gs = 0
                        zb, ab_ = 0, 1
                        for h in range(4):
                            c = h // 2
                            po = (h % 2) * 64
                            first = True
                            groups = [list(range(g0, min(g0 + 4, nk))) for g0 in range(0, nk, 4)]
                            for kbs in reversed(groups):
                                n = len(kbs)
                                wt = WT[gs % 2]
                                wtk = f"WT{gs % 2}"
                                gs += 1
                                for j, kb in enumerate(kbs):
                                    self.mm(B[zb][:, j * 128:(j + 1) * 128], SKT[po:po + 64, c, kb * 128:(kb + 1) * 128],
                                            SQT[po:po + 64, c, :], True, True, ["SKT", "SQT"], [Bk[zb]])
                                yield
                                zv = B[zb][:, 0:n * 128].rearrange("p (j t) -> p j t", t=128)
                                self.act(esb[:, 0:n, :], zv, AF.Exp, [Bk[zb]], ["esb"], scale=-0.125)
                                self.act(lp[:, 0:n, :], esb[:, 0:n, :], AF.Ln, ["esb"], ["lp"], bias=1.0)
                                self.stt(L1[:, 0:n, :], zv, -0.125, lp[:, 0:n, :], ALU.mult, ALU.subtract, [Bk[zb], "lp"], ["L1"])
                                if kbs[-1] == i:
                                    self.tt("pool", L1[:, n - 1, :], L1[:, n - 1, :], self.mask16, ALU.mult, ["L1", "cb16"], ["L1"])
                                yield
                                for j in reversed(range(n)):
                                    kb = kbs[j]
                                    self.mm(B[ab_][:, j * 128:(j + 1) * 128], self.tri16, L1[:, j, :], True, kb == i,
                                            ["cb16", "L1"], [Bk[ab_]])
                                    if kb < i:
                                        self.mm(B[ab_][:, j * 128:(j + 1) * 128], self.ones16, Ls[lsi % 2][:], False, True,
                                                ["cb16", f"Ls{lsi % 2}"], [Bk[ab_]])
                                        self.tt("pool", Ls[(lsi + 1) % 2][:], Ls[lsi % 2][:], L1[:, j, :], ALU.add,
                                                [f"Ls{lsi % 2}", "L1"], [f"Ls{(lsi + 1) % 2}"])
                                    else:
                                        self.cp("pool", Ls[(lsi + 1) % 2][:], L1[:, j, :], ["L1"], [f"Ls{(lsi + 1) % 2}"])
                                    lsi += 1
                                yield
                                av = B[ab_][:, 0:n * 128].rearrange("p (j t) -> p j t", t=128)
                                self.tt("dve", lg[:, 0:n, :], av, lp[:, 0:n, :], ALU.subtract, [Bk[ab_], "lp"], ["lg"])
                                self.act(wt[:, 0:n, :], lg[:, 0:n, :], AF.Exp, ["lg"], [wtk])
                                if kbs[-1] == i:
                                    self.tt("pool", wt[:, n - 1, :], wt[:, n - 1, :], self.mask16, ALU.mult, [wtk, "cb16"], [wtk])
                                yield
                                for j in reversed(range(n)):
                                    kb = kbs[j]
                                    self.mm(B[2][:, h * 64:(h + 1) * 64], wt[:, j, :], SV[:, kb, h * 64:(h + 1) * 64],
                                            first, kb == 0, [wtk, "SV"], [Bk[2]])
                                    first = False
                                yield
                        self.cp("act", mix[:, 768:1024], B[2][:, 0:256], [Bk[2]], ["mix"])

                    gens = [mla_gen(), sb_gen()]
                    alive = [True, True]
                    while alive[0] or alive[1]:
                        for gi_, cnt in ((0, 1), (1, 1)):
                            for _ in range(cnt):
                                if alive[gi_]:
                                    try:
                                        next(gens[gi_])
                                    except StopIteration:
                                        alive[gi_] = False

                    for cch in (0, 1, 2, 3, 6, 7):
                        self.tr(BT[:, cch, :], mix[:, cch * 128:(cch + 1) * 128], self.ident16, ["mix", "cb16"], ["BT"])
                    self.cp("act", mixT[:, 0:4, :], BT[:, 0:4, :], ["BT"], ["mixT"])
                    self.cp("act", mixT[:, 6:8, :], BT[:, 6:8, :], ["BT"], ["mixT"])
                    for hf in range(2):
                        for k in range(8):
                            self.mm(B[hf][:, :], mixT[:, k, :], wo[:, k, hf * 512:(hf + 1) * 512], k == 0, k == 7,
                                    ["mixT", "wo"], [Bk[hf]])
                    for hf in range(2):
                        cs = slice(hf * 512, (hf + 1) * 512)
                        self.tt("dve", tA[:, cs], B[hf][:, :], gp[:, cs], ALU.mult, [Bk[hf], "gp"], ["tA"])
                    self.stt(tA[:], xt[:], ALPHA, tA[:], ALU.mult, ALU.add, ["xt", "tA"], ["tA"])
                    self.ln_stats(tA[:], st, tB[:], ["tA"], "a")
                    self.act(tB[:], tA[:], AF.Identity, ["tA", "ast"], ["tB"], bias=st[:, 6:7], scale=st[:, 5:6])
                    self.tt("dve", tB[:], tB[:], lng[:], ALU.mult, ["tB", "lng"], ["tB"])
                    self.tt("pool", tB[:], tB[:], lnb[:], ALU.add, ["tB", "lnb"], ["tB"])
                    self.dma("sp", xdst[t0:t0 + 128, :], tB[:], ["tB"], [xdst_key])
                    self.pump_bg(4)

    def phase_b(self, l, xsrc, xsrc_key, xdst, xdst_key):
        nc, S = self.nc, self.S
        TT = 256
        NT = S // TT
        G = 4
        NQ = 8
        with ExitStack() as es:
            sb = lambda n, s, d: es.enter_context(nc.sbuf_tensor(f"{n}_b{l}", s, d))
            pst = lambda n, s, d=F32: es.enter_context(nc.psum_tensor(f"{n}_b{l}", s, d))
            wqs = [sb(f"wqs{j}", [128, 8, 256], BF16) for j in range(2)]
            kT = sb("kT", [128, 16, 128], BF16)
            lng = sb("lng", [128, D], F32)
            lnb = sb("lnb", [128, D], F32)
            scp = sb("scp", [128, D], F32)
            shp = sb("shp", [128, D], F32)
            gp = sb("gp", [128, D], F32)
            xt = sb("xt", [128, D], F32)
            tB = sb("tB", [128, D], F32)
            st = sb("st", [128, 8], F32)
            hb = sb("hb", [128, D], BF16)
            hT = [sb(f"hT{j}", [128, 8, TT], BF16) for j in range(2)]
            qT = sb("qT", [128, 16, 128], BF16)
            s12 = sb("s12", [128, 16, 128], F32)
            cand = s12[:].rearrange("p (h x) n -> p h (x n)", x=2)
            mr = sb("mr", [128, 8, 128], F32)
            mrc = sb("mrc", [128, 2, 256], F32)
            v12 = sb("v12", [128, 16, 16], F32)
            i12 = sb("i12", [128, 16, 16], U32)
            i12f = sb("i12f", [128, 16, 16], F32)
            ts_ = sb("ts", [128, 8, 16], F32)
            pos = sb("pos", [128, 8, 16], U32)
            pa = sb("pa", [128, 8, 16], U32)
            pb = sb("pb", [128, 8, 16], U32)
            paf = sb("paf", [128, 128], F32)
            pbf = sb("pbf", [128, 128], F32)
            oh = sb("oh", [128, 32, 16], F32)
            sel = sb("sel", [128, 3, 128], F32)
            ssum = sb("ssum", [128, 8], F32)
            selT = [sb(f"selT{j}", [128, 3, TT], F32) for j in range(2)]
            Pm = [sb(f"Pm{j}", [128, NQ, 128], BF16) for j in range(2)]
            i1i = sb("i1i", [128, TT], I32)
            i1a = sb("i1a", [128, TT], I32)
            i1af = sb("i1af", [128, 2, TT], F32)
            NP = 32
            Pa = sb("Pa", [128, NP, 8], BF16)
            Pb = sb("Pb", [128, NP, 16], BF16)
            Qm = [sb(f"Qm{j}", [128, NQ, 128], BF16) for j in range(2)]
            Wall = sb("Wall", [128, TT, 128], BF16)
            UW = [sb(f"UW{j}", [128, 8, G * 128], BF16) for j in range(3)]
            GV = 2
            VW = [sb(f"VW{j}", [128, GV, D], BF16) for j in range(2)]
            ga = [sb(f"ga{j}", [128, TT], BF16) for j in range(2)]
            wa = [sb(f"wa{j}", [128, TT], BF16) for j in range(2)]
            xd = sb("xd", [128, D], F32)
            tD = sb("tD", [128, D], F32)
            std = sb("std", [128, 8], F32)
            B = [pst(f"B{j}", [128, 512]) for j in range(7)]
            BT = pst("BT", [128, 8, 128], BF16)
            Bk = [f"B{j}" for j in range(7)]
            print("phase_b sbuf remaining", nc.sbuf_bytes_remaining)
            FB = 0
            AB = (1, 2)
            OB = 3

            self.dma("sp", kT[:], self.kT16[l].rearrange("p (g n) -> p g n", n=128), self.ck(("kT16", l)), ["kT"])
            self.dma("sp", lng[:], self.ln2_g[l].partition_broadcast(128), [], ["lng"])
            self.dma("sp", lnb[:], self.ln2_b[l].partition_broadcast(128), [], ["lnb"])
            iota16 = self.cst[:, C_IOTA16:C_IOTA16 + 16]
            iota128b = self.cst[:, C_IOTA:C_IOTA + 128]
            tiles = [(b, i) for b in range(self.nseq) for i in range(NT)]
            state = {"wcount": 0, "wqc": 0, "vcount": 0}

            def F1(n):
                b, i = tiles[n]
                par = n % 2
                hTp, hk = hT[par], f"hT{par}"
                sTp, sk = selT[par], f"selT{par}"
                for blk in range(TT // 128):
                    t0 = b * S + i * TT + blk * 128
                    bs = slice(blk * 128, (blk + 1) * 128)
                    self.dma("sp", xt[:], xsrc[t0:t0 + 128, :], [xsrc_key], ["xt"])
                    yield
                    self.ln_stats(xt[:], st, tB[:], ["xt"], "b")
                    yield
                    self.act(tB[:], xt[:], AF.Identity, ["xt", "bst"], ["tB"], bias=st[:, 6:7], scale=st[:, 5:6])
                    self.tt("dve", tB[:], tB[:], scp[:], ALU.mult, ["tB", "scp"], ["tB"])
                    self.tt("dve", hb[:], tB[:], shp[:], ALU.add, ["tB", "shp"], ["hb"])
                    yield
                    yield
                    yield
                    for k in range(8):
                        self.tr(BT[:, k, :], hb[:, k * 128:(k + 1) * 128], self.ident16, ["hb", "cb16"], ["BT"])
                    yield
                    self.cp("act", hTp[:, :, bs], BT[:], ["BT"], [hk])
                    yield
                    yield
                    def wq_load(cg):
                        wc = state["wqc"]
                        state["wqc"] += 1
                        self.dma("sp", wqs[wc % 2][:], self.wq16[l, :, cg * 256:(cg + 1) * 256].rearrange("(k p) c -> p k c", p=128),
                                 self.ck(("wq16", l)), [f"wqs{wc % 2}"])
                        return wqs[wc % 2], f"wqs{wc % 2}"
                    nxt_w = wq_load(0)
                    yield
                    for cg in range(8):
                        wqb, wqk = nxt_w
                        if cg + 1 < 8:
                            nxt_w = wq_load(cg + 1)
                        yield
                        for j in range(2):
                            sl_ = (cg % 2) * 2 + j
                            for k in range(8):
                                self.mm(B[FB][:, sl_ * 128:(sl_ + 1) * 128], wqb[:, k, j * 128:(j + 1) * 128], hTp[:, k, bs],
                                        k == 0, k == 7, [wqk, hk], [Bk[FB]])
                        yield
                        if cg % 2 == 1:
                            c4 = cg // 2
                            self.cp("act", qT[:, c4 * 4:(c4 + 1) * 4, :], B[FB][:, :].rearrange("p (j t) -> p j t", t=128),
                                    [Bk[FB]], ["qT"])
                            yield
                    yield
                    for bq in range(4):
                        for j in range(4):
                            gidx = bq * 4 + j
                            half, h = gidx // 8, gidx % 8
                            self.mm(B[FB][:, j * 128:(j + 1) * 128], qT[:, 2 * h + half, :], kT[:, gidx, :],
                                    True, True, ["qT", "kT"], [Bk[FB]])
                        yield
                        self.cp("act", s12[:, bq * 4:(bq + 1) * 4, :], B[FB][:, :].rearrange("p (j n) -> p j n", n=128),
                                [Bk[FB]], ["s12"])
                        yield
                    yield
                    for g0 in (0, 8):
                        gs = range(g0, g0 + 8)
                        for g in gs:
                            self.P.op("dve", (lambda g=g: lambda e: e.max(out=v12[:, g, 0:8], in_=s12[:, g, :]))(), ["s12"], [("v12", g)])
                        for g in gs:
                            self.P.op("dve", (lambda g=g: lambda e: e.max_index(out=i12[:, g, 0:8], in_max=v12[:, g, 0:8], in_values=s12[:, g, :]))(),
                                      ["s12", ("v12", g)], [("i12", g)])
                        yield
                        for g in gs:
                            self.P.op("dve", (lambda g=g: lambda e: e.match_replace(out=mr[:, g % 8, :], in_to_replace=v12[:, g, 0:8], in_values=s12[:, g, :], imm_value=-1e30))(),
                                      ["s12", ("v12", g)], [("mr", g % 8)])
                        yield
                        for g in gs:
                            self.P.op("dve", (lambda g=g: lambda e: e.max(out=v12[:, g, 8:16], in_=mr[:, g % 8, :]))(), [("mr", g % 8)], [("v12b", g)])
                        for g in gs:
                            self.P.op("dve", (lambda g=g: lambda e: e.max_index(out=i12[:, g, 8:16], in_max=v12[:, g, 8:16], in_values=mr[:, g % 8, :]))(),
                                      [("mr", g % 8), ("v12b", g)], [("i12b", g)])
                        yield
                    vkeys = [("v12", g) for g in range(16)] + [("v12b", g) for g in range(16)]
                    ikeys = [("i12", g) for g in range(16)] + [("i12b", g) for g in range(16)]
                    self.cp("dve", i12f[:], i12[:], ikeys, ["i12f"])
                    self.tt("dve", cand.rearrange("p h (a b) -> p h a b", b=16),
                            v12[:, 0:8, :].unsqueeze(3).to_broadcast([128, 8, 16, 16]),
                            v12[:, 8:16, :].unsqueeze(2).to_broadcast([128, 8, 16, 16]), ALU.add, vkeys, ["s12"])
                    yield
                    for h0 in (0, 2, 4, 6):
                        hs = range(h0, h0 + 2)
                        for h in hs:
                            self.P.op("dve", (lambda h=h: lambda e: e.max(out=ts_[:, h, 0:8], in_=cand[:, h, :]))(), ["s12"], [("ts", h)])
                        for h in hs:
                            self.P.op("dve", (lambda h=h: lambda e: e.max_index(out=pos[:, h, 0:8], in_max=ts_[:, h, 0:8], in_values=cand[:, h, :]))(),
                                      ["s12", ("ts", h)], [("pos", h)])
                        for h in hs:
                            self.P.op("dve", (lambda h=h: lambda e: e.match_replace(out=mrc[:, h % 2, :], in_to_replace=ts_[:, h, 0:8], in_values=cand[:, h, :], imm_value=-1e30))(),
                                      ["s12", ("ts", h)], [("mrc", h % 2)])
                        yield
                        for h in hs:
                            self.P.op("dve", (lambda h=h: lambda e: e.max(out=ts_[:, h, 8:16], in_=mrc[:, h % 2, :]))(), [("mrc", h % 2)], [("tsb", h)])
                        for h in hs:
                            self.P.op("dve", (lambda h=h: lambda e: e.max_index(out=pos[:, h, 8:16], in_max=ts_[:, h, 8:16], in_values=mrc[:, h % 2, :]))(),
                                      [("mrc", h % 2), ("tsb", h)], [("posb", h)])
                        yield
                    tkeys = [("ts", h) for h in range(8)] + [("tsb", h) for h in range(8)]
                    pkeys = [("pos", h) for h in range(8)] + [("posb", h) for h in range(8)]
                    self.ts("dve", pa[:], pos[:], 4, None, ALU.logical_shift_right, None, pkeys, ["pa"])
                    self.ts("dve", pb[:], pos[:], 15, None, ALU.bitwise_and, None, pkeys, ["pb"])
                    self.cp("dve", paf[:].rearrange("p (h k) -> p h k", k=16), pa[:], ["pa"], ["paf"])
                    self.cp("dve", pbf[:].rearrange("p (h k) -> p h k", k=16), pb[:], ["pb"], ["pbf"])
                    yield
                    for which, pf, pk in ((0, paf, "paf"), (1, pbf, "pbf")):
                        for hh in range(4):
                            hs = slice(hh * 32, (hh + 1) * 32)
                            self.tt("dve", oh[:], pf[:, hs].unsqueeze(2).to_broadcast([128, 32, 16]),
                                    iota16.unsqueeze(1).to_broadcast([128, 32, 16]), ALU.is_equal, [pk, "cst"], ["oh"])
                            self.tt("dve", oh[:].rearrange("p (h k) a -> p h k a", k=16), oh[:].rearrange("p (h k) a -> p h k a", k=16),
                                    i12f[:, which * 8 + hh * 2:which * 8 + hh * 2 + 2, :].unsqueeze(2).to_broadcast([128, 2, 16, 16]), ALU.mult,
                                    ["oh", "i12f"], ["oh"])
                            yield
                            self.red(sel[:, which, hs], oh[:], ["oh"], ["sel"])
                            yield
                    tv = ts_[:]
                    g3 = sel[:, 2, :].rearrange("p (h k) -> p h k", k=16)
                    self.tt("dve", g3, tv, tv[:, :, 0:1].to_broadcast([128, 8, 16]), ALU.subtract, tkeys, ["sel"])
                    self.act(sel[:, 2, :], sel[:, 2, :], AF.Exp, ["sel"], ["sel"])
                    yield
                    self.red(ssum[:], g3, ["sel"], ["ssum"])
                    self.P.op("dve", lambda e: e.reciprocal(out=ssum[:], in_=ssum[:]), ["ssum"], ["ssum"])
                    self.tt("dve", g3, g3, ssum[:].unsqueeze(2).to_broadcast([128, 8, 16]), ALU.mult, ["sel", "ssum"], ["sel"])
                    yield
                    yield
                    for j in range(3):
                        self.tr(B[FB][:, j * 128:(j + 1) * 128], sel[:, j, :], self.identf, ["sel", "cst"], [Bk[FB]])
                    yield
                    self.cp("act", sTp[:, :, bs], B[FB][:, 0:384].rearrange("p (j t) -> p j t", t=128), [Bk[FB]], [sk])
                    yield

            def F2D(n, gen, prev_tail=None):
                b, i = tiles[n]
                par = n % 2
                hTp, hk = hT[par], f"hT{par}"
                sTp, sk = selT[par], f"selT{par}"

                def pull(k=1):
                    if gen is None:
                        return
                    for _ in range(k):
                        try:
                            next(gen)
                        except StopIteration:
                            return
                ib = iota128b.unsqueeze(1).to_broadcast([128, NQ, 128])
                wbanks = (FB, AB[0], AB[1])
                rr = 0
                self.cp("dve", i1i[:], sTp[:, 0, :], [sk], ["i1i"])
                self.ts("dve", i1a[:], i1i[:], 4, None, ALU.logical_shift_right, None, ["i1i"], ["i1a"])
                self.cp("dve", i1af[:, 0, :], i1a[:], ["i1a"], ["i1af"])
                self.ts("dve", i1a[:], i1i[:], 15, None, ALU.bitwise_and, None, ["i1i"], ["i1a"])
                self.cp("dve", i1af[:, 1, :], i1a[:], ["i1a"], ["i1af"])
                for qd in range(TT // NQ):
                    tq = slice(qd * NQ, (qd + 1) * NQ)
                    pm, qm = Pm[qd % 2], Qm[qd % 2]
                    pmk, qmk = f"Pm{qd % 2}", f"Qm{qd % 2}"
                    if (qd * NQ) % NP == 0:
                        tp_ = slice(qd * NQ, qd * NQ + NP)
                        self.tt("dve", Pa[:], iota16[:, 0:8].unsqueeze(1).to_broadcast([128, NP, 8]),
                                i1af[:, 0, tp_].unsqueeze(2).to_broadcast([128, NP, 8]), ALU.is_equal, ["cst", "i1af"], ["Pa"])
                        self.tt("dve", Pa[:], Pa[:], sTp[:, 2, tp_].unsqueeze(2).to_broadcast([128, NP, 8]), ALU.mult,
                                ["Pa", sk], ["Pa"])
                        self.tt("dve", Pb[:], iota16.unsqueeze(1).to_broadcast([128, NP, 16]),
                                i1af[:, 1, tp_].unsqueeze(2).to_broadcast([128, NP, 16]), ALU.is_equal, ["cst", "i1af"], ["Pb"])
                    o_ = (qd * NQ) % NP
                    self.tt("dve", qm[:], ib, sTp[:, 1, tq].unsqueeze(2).to_broadcast([128, NQ, 128]), ALU.is_equal,
                            ["cst", sk], [qmk])
                    self.tt("pool", pm[:].rearrange("p t (a b) -> p t a b", b=16),
                            Pa[:, o_:o_ + NQ, :].unsqueeze(3).to_broadcast([128, NQ, 8, 16]),
                            Pb[:, o_:o_ + NQ, :].unsqueeze(2).to_broadcast([128, NQ, 8, 16]), ALU.mult, ["Pa", "Pb"], [pmk])
                    if prev_tail is not None:
                        try:
                            next(prev_tail)
                        except StopIteration:
                            prev_tail = None
                    for t4 in range(NQ // 4):
                        bank = wbanks[rr % 3]
                        rr += 1
                        for j in range(4):
                            tl = t4 * 4 + j
                            self.mm(B[bank][:, j * 128:(j + 1) * 128], qm[:, tl, :], pm[:, tl, :], True, True,
                                    [qmk, pmk], [Bk[bank]])
                        tg = qd * NQ + t4 * 4
                        self.cp("act", Wall[:, tg:tg + 4, :], B[bank][:, :].rearrange("p (j i) -> p j i", i=128),
                                [Bk[bank]], ["Wall"])
                if prev_tail is not None:
                    for _ in prev_tail:
                        pass
                bufs = {}

                def emit_u(ch):
                    g, cc = ch // G, ch % G
                    if cc == 0:
                        wc = state["wcount"]
                        state["wcount"] += 1
                        ub, uk = UW[wc % 3], f"UW{wc % 3}"
                        self.dma("sp", ub[:], self.uG[l, g], [("uG", l, g)], [uk])
                        bufs[("u", g)] = (ub, uk)
                    if ch % GV == 0:
                        vc = state["vcount"]
                        state["vcount"] += 1
                        vb, vk = VW[vc % 2], f"VW{vc % 2}"
                        gv = ch // GV
                        self.dma("sp", vb[:], self.vG[l, gv], [("vG", l, gv)], [vk])
                        bufs[("v", gv)] = (vb, vk)
                    ub, uk = bufs[("u", g)]
                    ab_ = AB[ch % 2]
                    gb, wb = ga[ch % 2], wa[ch % 2]
                    gk, wk_ = f"ga{ch % 2}", f"wa{ch % 2}"
                    for k in range(8):
                        self.mm(B[ab_][:, 0:TT], ub[:, k, cc * 128:(cc + 1) * 128], hTp[:, k, :], k == 0, k == 7,
                                [uk, hk], [Bk[ab_]])
                    self.act(gb[:], B[ab_][:, 0:TT], AF.Gelu, [Bk[ab_]], [gk])
                    self.tt("pool", wb[:], gb[:], Wall[:, :, ch], ALU.mult, [gk, "Wall"], [wk_])

                def emit_v(ch):
                    vb, vk = bufs[("v", ch // GV)]
                    cc = ch % GV
                    wb, wk_ = wa[ch % 2], f"wa{ch % 2}"
                    for sbk in range(TT // 128):
                        for hf in range(2):
                            ob = OB + sbk * 2 + hf
                            self.mm(B[ob][:, :], wb[:, sbk * 128:(sbk + 1) * 128], vb[:, cc, hf * 512:(hf + 1) * 512],
                                    ch == 0, ch == 127, [wk_, vk], [Bk[ob]])

                SKEW = 1
                for ch in range(128 + SKEW):
                    if ch < 128:
                        emit_u(ch)
                    if ch >= SKEW:
                        emit_v(ch - SKEW)
                    if ch >= 3:
                        pull(2)
                pull(10000)

                def tail():
                    for sbk in range(TT // 128):
                        t0 = b * S + i * TT + sbk * 128
                        self.dma("sp", xd[:], xsrc[t0:t0 + 128, :], [xsrc_key], ["xd"])
                        yield
                        for hf in range(2):
                            cs = slice(hf * 512, (hf + 1) * 512)
                            ob = OB + sbk * 2 + hf
                            self.tt("dve", tD[:, cs], B[ob][:, :], gp[:, cs], ALU.mult, [Bk[ob], "gp"], ["tD"])
                            yield
                        self.stt(xd[:], xd[:], ALPHA, tD[:], ALU.mult, ALU.add, ["xd", "tD"], ["xd"])
                        yield
                        self.ln_stats(xd[:], std, tD[:], ["xd"], "d")
                        yield
                        self.act(tD[:], xd[:], AF.Identity, ["xd", "dst"], ["tD"], bias=std[:, 6:7], scale=std[:, 5:6])
                        yield
                        self.tt("dve", tD[:], tD[:], lng[:], ALU.mult, ["tD", "lng"], ["tD"])
                        yield
                        self.tt("pool", tD[:], tD[:], lnb[:], ALU.add, ["tD", "lnb"], ["tD"])
                        yield
                        self.dma("sp", xdst[t0:t0 + 128, :], tD[:], ["tD"], [xdst_key])
                        yield
                return tail()

            def load_seq(b):
                self.dma("sp", shp[:], self.modd[l, b, 3 * D:4 * D].partition_broadcast(128), [("modd", l)], ["shp"])
                self.dma("sp", scp[:], self.modd[l, b, 4 * D:5 * D].partition_broadcast(128), [("modd", l)], ["scp"])
                self.dma("sp", gp[:], self.modd[l, b, 5 * D:6 * D].partition_broadcast(128), [("modd", l)], ["gp"])

            load_seq(0)
            for _ in F1(0):
                pass
            tl = None
            for n in range(len(tiles)):
                nxt = None
                if n + 1 < len(tiles) and tiles[n + 1][0] == tiles[n][0]:
                    nxt = F1(n + 1)
                tl = F2D(n, nxt, tl)
                if nxt is None:
                    for _ in tl:
                        pass
                    tl = None
                    if n + 1 < len(tiles):
                        load_seq(tiles[n + 1][0])
                        for _ in F1(n + 1):
                            pass

    def build(self):
        self.declare()
        with ExitStack() as es:
            self.load_consts(es)
            self.plan_casts()
            cur, curk = self.x, "x"
            for l in range(self.layers):
                self.flush_casts(("adaw16", l), ("win16", l), ("wuq16", l), ("wo16", l), ("wq16", l), ("kT16", l))
                self.P.barrier()
                self.mod_phase(l)
                self.P.barrier()
                last = (l == self.layers - 1)
                if self.stop_after == ("a", l):
                    self.phase_a(l, cur, curk, self.out, "out")
                    break
                self.bg = self.bg_stream(l)
                self.phase_a(l, cur, curk, self.x1, "x1")
                self.pump_bg(100000)
                self.P.barrier()
                dst, dstk = (self.out, "out") if last else (self.x2, "x2")
                self.phase_b(l, self.x1, "x1", dst, dstk)
                cur, curk = dst, dstk
            outs = [o for o in reversed(self.P.ops) if o.is_dma]
            self.P.op("sp", None, extra_deps=outs)
            self.P.emit(self.nc)
        return self.nc


def host_inputs(inputs, nseq, core, S):
    f = lambda a: np.ascontiguousarray(np.asarray(a))
    b0 = core * nseq
    m = {}
    m["x"] = f(inputs["x"][b0:b0 + nseq]).reshape(nseq * S, D)
    m["c"] = f(inputs["c"][b0:b0 + nseq])
    m["pos"] = f(inputs["positions"][b0:b0 + nseq]).astype(np.int32)
    return m


_SHARED = {}


def shared_inputs(inputs):
    f = lambda a: np.ascontiguousarray(np.asarray(a, dtype=np.float32))
    m = {}
    for k in ("ada_w", "ada_b", "w_in", "q_norm_g", "kv_norm_g", "w_uq", "conv_w", "w_o", "ln1_g", "ln1_b",
              "peer_wq", "peer_v", "ln2_g", "ln2_b"):
        m[k] = f(inputs[k])
    wukv = np.asarray(inputs["w_ukv"], dtype=np.float32).reshape(L, 128, 8, 128)
    m["w_ukv_k"] = f(wukv[:, :, :, 0:64].reshape(L, 128, 512))
    m["w_ukv_v"] = f(wukv[:, :, :, 64:128].reshape(L, 128, 512))
    k1 = np.asarray(inputs["peer_k1"], dtype=np.float32)
    k2 = np.asarray(inputs["peer_k2"], dtype=np.float32)
    kk = np.stack([k1, k2], axis=1).reshape(L, 16, 128, 128)
    m["peer_kT"] = f(kk.transpose(0, 3, 1, 2).reshape(L, 128, 2048))
    m["peer_uT"] = f(np.asarray(inputs["peer_u"], dtype=np.float32).transpose(0, 2, 1))
    m["consts"] = make_consts()
    return m


def kernel(**inputs):
    x = np.asarray(inputs["x"])
    Bt, S, _ = x.shape
    ncores = 8
    nseq = Bt // ncores
    kb = K(nseq, S)
    nc = kb.build()
    sh = shared_inputs(inputs)
    in_maps = []
    for core in range(ncores):
        m = dict(sh)
        m.update(host_inputs(inputs, nseq, core, S))
        in_maps.append(m)
    res = run_bass_kernel_spmd(nc, in_maps, core_ids=list(range(ncores)))
    out = np.concatenate([np.asarray(r["out"]).reshape(nseq, S, D) for r in res.results], axis=0)
    return out.astype(np.float32)
```
